# Optimizing a Trainium2 kernel written in Bass

```python
import jax
import jax.numpy as jnp
from jax import lax
import numpy as np

D_MODEL = 2048
BATCH = 4
SEQ = 2048
DEPTH = 4
DEC_BATCH = 8
DEC_SEQ = 8
PAST_LEN = 16384
PAGE_SIZE = 128

NORM_EPS = 1e-6
N_AB = (DEPTH + 1) // 2
N_C = DEPTH // 2
A_GROUPS = ((128, 1), (512, 4), (2048, 16))
A_HEADS = 8
A_HEAD_DIM = 128
A_WIDTH = A_HEADS * A_HEAD_DIM
B_HEADS = 8
B_KEY_DIM = 128
B_VAL_DIM = 128
B_WIDTH = B_HEADS * B_VAL_DIM
B_CHUNK = 64
AB_SIZES = (A_WIDTH,) * (3 * len(A_GROUPS)) + (B_HEADS * B_KEY_DIM,) * 2 + (B_WIDTH,) * 2
AB_IN = sum(AB_SIZES)
C_HEAD_DIM = 64
C_HEADS = D_MODEL // C_HEAD_DIM
C_DECAY_LORA = max(32, int(round(1.8 * D_MODEL ** 0.5 / 32)) * 32)
C_ICLR_LORA = max(32, int(round(1.8 * D_MODEL ** 0.5 / 32)) * 32)
C_GATE_LORA = max(32, int(round(0.6 * D_MODEL ** 0.8 / 32)) * 32)
C_GN_EPS = 64e-5
FFN_HIDDEN = -((-8 * D_MODEL) // (3 * 256)) * 256

kernel_name = 'dilated_hgrn2_rwkv7_hybrid_step'


def _rmsnorm(x, gain):
    xf = x.astype(jnp.float32)
    y = xf * lax.rsqrt(jnp.mean(xf * xf, axis=-1, keepdims=True) + NORM_EPS)
    return (y * gain.astype(jnp.float32)).astype(x.dtype)


def _alibi_slopes(n):
    return jnp.exp2(-8.0 * jnp.arange(1, n + 1, dtype=jnp.float32) / n)


def _dilated_window_prompt(q, k, v, window, dilation, slopes):
    bn, t_len, nh, hd = q.shape
    nk = window // dilation
    m_len = t_len // dilation
    blk = nk
    nb = -(-m_len // blk)
    m_pad = nb * blk

    def to_residue_blocks(t):
        t = t.reshape(bn, m_len, dilation, nh, hd).transpose(0, 2, 1, 3, 4)
        t = jnp.pad(t, ((0, 0), (0, 0), (0, m_pad - m_len), (0, 0), (0, 0)))
        return t.reshape(bn, dilation, nb, blk, nh, hd)

    def with_previous(t):
        prev = jnp.pad(t, ((0, 0), (0, 0), (1, 0), (0, 0), (0, 0), (0, 0)))[:, :, :-1]
        return jnp.concatenate([prev, t], axis=3)

    qb = to_residue_blocks(q)
    kb = with_previous(to_residue_blocks(k))
    vb = with_previous(to_residue_blocks(v))
    qi = jnp.arange(blk)[:, None] + blk
    ki = jnp.arange(2 * blk)[None, :]
    dist = qi - ki
    key_m = (jnp.arange(nb) * blk)[:, None, None] + ki[None] - blk
    valid = (dist >= 0)[None] & (dist <= nk)[None] & (key_m >= 0)
    s = jnp.einsum('brnqhd,brnkhd->brnhqk', qb, kb).astype(jnp.float32) * (hd ** -0.5)
    s = s - slopes[:, None, None] * (dist * dilation).astype(jnp.float32)
    s = jnp.where(valid[:, None], s, -jnp.inf)
    mx = jnp.max(s, axis=-1, keepdims=True)
    p = jnp.exp(s - mx)
    den = jnp.sum(p, axis=-1, keepdims=True)
    o = jnp.einsum('brnhqk,brnkhd->brnqhd', p / den, vb.astype(jnp.float32))
    lse = (mx + jnp.log(den))[..., 0]
    o = o.reshape(bn, dilation, m_pad, nh, hd)[:, :, :m_len].transpose(0, 2, 1, 3, 4).reshape(bn, t_len, nh, hd)
    lse = lse.transpose(0, 1, 2, 4, 3).reshape(bn, dilation, m_pad, nh)[:, :, :m_len]
    lse = lse.transpose(0, 2, 1, 3).reshape(bn, t_len, nh)
    return o, lse


def _dilated_window_sample(q, k_all, v_all, window, dilation, slopes, n_buf):
    l_new, hd = q.shape[1], q.shape[-1]
    nk = window // dilation
    steps = jnp.arange(nk + 1)
    idx = n_buf + jnp.arange(l_new)[:, None] - steps[None, :] * dilation
    valid = idx >= 0
    idx = jnp.maximum(idx, 0)
    kg = k_all[:, idx]
    vg = v_all[:, idx]
    s = jnp.einsum('blhd,bljhd->blhj', q, kg).astype(jnp.float32) * (hd ** -0.5)
    s = s - slopes[:, None] * (steps * dilation).astype(jnp.float32)[None]
    s = jnp.where(valid[:, None, :], s, -jnp.inf)
    mx = jnp.max(s, axis=-1, keepdims=True)
    p = jnp.exp(s - mx)
    den = jnp.sum(p, axis=-1, keepdims=True)
    o = jnp.einsum('blhj,bljhd->blhd', p / den, vg.astype(jnp.float32))
    lse = (mx + jnp.log(den))[..., 0]
    return o, lse


def _merge_by_denominator(outs, lses):
    w = jax.nn.softmax(jnp.stack(lses, axis=0), axis=0)
    return jnp.einsum('gbth,gbthd->bthd', w, jnp.stack(outs, axis=0))


def _gla_chunked(q, k, v, g, s0, chunk):
    bn, t_len, nh, _ = q.shape
    c = min(chunk, t_len)
    n = -(-t_len // c)
    t_pad = n * c

    def to_chunks(t):
        t = jnp.pad(t.astype(jnp.float32), ((0, 0), (0, t_pad - t_len), (0, 0), (0, 0)))
        return t.reshape(bn, n, c, nh, -1).transpose(1, 0, 3, 2, 4)

    qc, kc, vc, gc = (to_chunks(t) for t in (q, k, v, g))
    causal = jnp.tril(jnp.ones((c, c), dtype=bool))[:, :, None]

    def step(s, inp):
        qi, ki, vi, gi = inp
        b = jnp.cumsum(gi, axis=2)
        o_inter = jnp.einsum('bhtk,bhkv->bhtv', qi * jnp.exp(b), s)
        diff = b[:, :, :, None, :] - b[:, :, None, :, :]
        decay = jnp.where(causal, jnp.exp(jnp.where(causal, diff, 0.0)), 0.0)
        att = jnp.einsum('bhtk,bhtsk,bhsk->bhts', qi, decay, ki)
        o = o_inter + jnp.einsum('bhts,bhsv->bhtv', att, vi)
        b_last = b[:, :, -1:, :]
        s_new = jnp.exp(b_last[:, :, 0, :, None]) * s + jnp.einsum('bhsk,bhsv->bhkv', ki * jnp.exp(b_last - b), vi)
        return s_new, o

    s_fin, o = lax.scan(step, s0.astype(jnp.float32), (qc, kc, vc, gc))
    o = o.transpose(1, 0, 3, 2, 4).reshape(bn, t_pad, nh, -1)[:, :t_len]
    return o, s_fin


def _ab_mixer(h, w_in, w_out, lower_bound, b_gain, a_bufs, b_state):
    bn, t_len, _ = h.shape
    proj = h @ w_in
    parts = jnp.split(proj, [int(c) for c in np.cumsum(AB_SIZES)[:-1]], axis=-1)
    slopes = _alibi_slopes(A_HEADS)
    outs, lses, new_bufs = [], [], []
    for gi, (window, dil) in enumerate(A_GROUPS):
        q, k, v = (t.reshape(bn, t_len, A_HEADS, A_HEAD_DIM) for t in parts[3 * gi:3 * gi + 3])
        kv_new = jnp.stack([k, v], axis=2)
        if a_bufs is None:
            o, lse = _dilated_window_prompt(q, k, v, window, dil, slopes)
            new_bufs.append(kv_new[:, -min(window, t_len):])
        else:
            n_buf = a_bufs[gi].shape[1]
            kv_all = jnp.concatenate([a_bufs[gi].astype(kv_new.dtype), kv_new], axis=1)
            o, lse = _dilated_window_sample(q, kv_all[:, :, 0], kv_all[:, :, 1], window, dil, slopes, n_buf)
            new_bufs.append(kv_all[:, t_len:])
        outs.append(o)
        lses.append(lse)
    a_out = _merge_by_denominator(outs, lses).reshape(bn, t_len, A_WIDTH).astype(h.dtype)

    bq, bf, bi, bg = parts[-4:]
    q = jax.nn.silu(bq.astype(jnp.float32)).reshape(bn, t_len, B_HEADS, B_KEY_DIM)
    lb = lower_bound.reshape(B_HEADS, B_KEY_DIM)
    fgate = lb + (1.0 - lb) * jax.nn.sigmoid(bf.astype(jnp.float32).reshape(bn, t_len, B_HEADS, B_KEY_DIM))
    o, s_b = _gla_chunked(q, 1.0 - fgate, bi.reshape(bn, t_len, B_HEADS, B_VAL_DIM), jnp.log(fgate), b_state, B_CHUNK)
    o = _rmsnorm(o, b_gain.reshape(B_HEADS, B_VAL_DIM)) * jax.nn.silu(bg.astype(jnp.float32).reshape(bn, t_len, B_HEADS, B_VAL_DIM))
    b_out = o.reshape(bn, t_len, B_WIDTH).astype(h.dtype)
    y = jnp.concatenate([a_out, b_out], axis=-1) @ w_out
    return y, tuple(new_bufs), s_b


def _rwkv7_scan(r, decay, k, v, kk, a, s0):
    seq = tuple(t.transpose(1, 0, 2, 3) for t in (r, decay, k, v, kk, a))

    def step(s, inp):
        r_t, w_t, k_t, v_t, kk_t, a_t = inp
        sa = jnp.einsum('bhvk,bhk->bhv', s, -kk_t)
        s = s * w_t[:, :, None, :] + sa[..., None] * (kk_t * a_t)[:, :, None, :] + v_t[..., None] * k_t[:, :, None, :]
        return s, jnp.einsum('bhvk,bhk->bhv', s, r_t)

    s_fin, o = lax.scan(step, s0.astype(jnp.float32), seq)
    return o.transpose(1, 0, 2, 3), s_fin


def _rwkv7_mixer(h, shift, wkv, c_mu, c_w_rkv, c_w0, c_w1, c_w2, c_a0, c_a1, c_a2, c_g1, c_g2,
                 c_k_k, c_k_a, c_r_k, c_ln_w, c_ln_b, c_w_out):
    bn, t_len, d = h.shape
    prev = jnp.concatenate([shift[:, None].astype(h.dtype), h[:, :-1]], axis=1)
    xx = prev - h
    xr, xw, xk, xv, xa, xg = (h + xx * c_mu[j] for j in range(6))
    r, k, v = jnp.einsum('nbtd,nde->nbte', jnp.stack([xr, xk, xv]), c_w_rkv)
    log_decay = -jnp.exp(-jax.nn.softplus(-(c_w0 + jnp.tanh(xw @ c_w1) @ c_w2).astype(jnp.float32)) - 0.5)
    a = jax.nn.sigmoid((c_a0 + (xa @ c_a1) @ c_a2).astype(jnp.float32))
    gate = (jax.nn.sigmoid(xg @ c_g1) @ c_g2).astype(jnp.float32)

    def heads(t):
        return t.astype(jnp.float32).reshape(bn, t_len, C_HEADS, C_HEAD_DIM)

    kk = heads(k * c_k_k)
    kk = kk / jnp.maximum(jnp.sqrt(jnp.sum(kk * kk, axis=-1, keepdims=True)), 1e-12)
    k2 = k.astype(jnp.float32) * (1.0 + (a - 1.0) * c_k_a.astype(jnp.float32))
    r, k2, v, a, decay = (heads(t) for t in (r, k2, v, a, jnp.exp(log_decay)))
    o, s_fin = _rwkv7_scan(r, decay, k2, v, kk, a, wkv)
    mean = jnp.mean(o, axis=-1, keepdims=True)
    var = jnp.mean(jnp.square(o - mean), axis=-1, keepdims=True)
    o = (o - mean) * lax.rsqrt(var + C_GN_EPS) * heads(c_ln_w) [0, 0] + heads(c_ln_b)[0, 0] if False else \
        (o - mean) * lax.rsqrt(var + C_GN_EPS) * c_ln_w.astype(jnp.float32).reshape(C_HEADS, C_HEAD_DIM) + c_ln_b.astype(jnp.float32).reshape(C_HEADS, C_HEAD_DIM)
    o = o + jnp.sum(r * k2 * c_r_k.astype(jnp.float32), axis=-1, keepdims=True) * v
    y = (o.reshape(bn, t_len, d) * gate).astype(h.dtype) @ c_w_out
    return y, s_fin, h[:, -1]


def _swiglu(h, w_up, w_down):
    gate, up = jnp.split(h @ w_up, 2, axis=-1)
    return (jax.nn.silu(gate) * up) @ w_down


C_PARAM_NAMES = ('c_mu', 'c_w_rkv', 'c_w0', 'c_w1', 'c_w2', 'c_a0', 'c_a1', 'c_a2', 'c_g1', 'c_g2',
                 'c_k_k', 'c_k_a', 'c_r_k', 'c_ln_w', 'c_ln_b', 'c_w_out')


def _trunk(x, a_caches, b_states, c_wkv, c_shift, p):
    bn = x.shape[0]
    lb_soft = jax.nn.softmax(p['b_lower_bounds'].astype(jnp.float32), axis=0)
    lower_bounds = jnp.cumsum(lb_soft, axis=0) - lb_soft[0]
    new_a = tuple([] for _ in A_GROUPS)
    new_b, new_wkv, new_shift = [], [], []
    for layer in range(DEPTH):
        gains = p['norm_gains'][layer]
        li = layer // 2
        h = _rmsnorm(x, gains[0])
        if layer % 2 == 0:
            bufs = None if a_caches is None else tuple(c[li] for c in a_caches)
            s0 = jnp.zeros((bn, B_HEADS, B_KEY_DIM, B_VAL_DIM), jnp.float32) if b_states is None else b_states[li]
            mix, kv_bufs, s_b = _ab_mixer(h, p['w_in_ab'][li], p['w_out_ab'][li], lower_bounds[li],
                                          p['b_norm_gain'][li], bufs, s0)
            for lst, kv in zip(new_a, kv_bufs):
                lst.append(kv)
            new_b.append(s_b)
        else:
            wkv0 = jnp.zeros((bn, C_HEADS, C_HEAD_DIM, C_HEAD_DIM), jnp.float32) if c_wkv is None else c_wkv[li]
            sh0 = jnp.zeros((bn, D_MODEL), x.dtype) if c_shift is None else c_shift[li]
            cp = {name: p[name][li] for name in C_PARAM_NAMES}
            mix, s_wkv, s_shift = _rwkv7_mixer(h, sh0, wkv0, **cp)
            new_wkv.append(s_wkv)
            new_shift.append(s_shift)
        x = x + _rmsnorm(mix, gains[1])
        h = _rmsnorm(x, gains[2])
        x = x + _rmsnorm(_swiglu(h, p['w_ffn_up'][layer], p['w_ffn_down'][layer]), gains[3])
    a_states = tuple(jnp.stack(lst) for lst in new_a)
    return x, a_states, jnp.stack(new_b), jnp.stack(new_wkv), jnp.stack(new_shift)


def setup_inputs(seed: int = 0) -> dict:
    key = jax.random.key(seed)
    keys = iter(jax.random.split(key, 48))

    def nrm(shape, scale):
        return jax.random.normal(next(keys), shape, jnp.float32) * scale

    def unif(shape, lo, hi):
        return jax.random.uniform(next(keys), shape, jnp.float32, lo, hi)

    d = D_MODEL
    inputs = {
        'x_prompt': nrm((BATCH, SEQ, d), 1.0),
        'x_sample': nrm((DEC_BATCH, DEC_SEQ, d), 1.0),
    }
    for gi, (window, _) in enumerate(A_GROUPS):
        inputs['cache_a%d_kv' % (gi + 1)] = nrm((N_AB, DEC_BATCH, min(window, PAST_LEN), 2, A_HEADS, A_HEAD_DIM), 1.0)
    inputs['state_b'] = nrm((N_AB, DEC_BATCH, B_HEADS, B_KEY_DIM, B_VAL_DIM), 0.5)
    inputs['state_c_wkv'] = nrm((N_C, DEC_BATCH, C_HEADS, C_HEAD_DIM, C_HEAD_DIM), 0.5)
    inputs['state_c_shift'] = nrm((N_C, DEC_BATCH, d), 1.0)
    inputs['norm_gains'] = 1.0 + nrm((DEPTH, 4, d), 0.05)
    inputs['w_in_ab'] = nrm((N_AB, d, AB_IN), d ** -0.5)
    inputs['w_out_ab'] = nrm((N_AB, A_WIDTH + B_WIDTH, d), (A_WIDTH + B_WIDTH) ** -0.5)
    inputs['b_lower_bounds'] = 1.0 + nrm((N_AB, B_HEADS * B_KEY_DIM), 0.3)
    inputs['b_norm_gain'] = 1.0 + nrm((N_AB, B_WIDTH), 0.05)
    inputs['c_mu'] = unif((N_C, 6, d), 0.0, 1.0)
    inputs['c_w_rkv'] = nrm((N_C, 3, d, d), d ** -0.5)
    inputs['c_w0'] = unif((N_C, d), -3.0, 1.0)
    inputs['c_w1'] = nrm((N_C, d, C_DECAY_LORA), d ** -0.5)
    inputs['c_w2'] = nrm((N_C, C_DECAY_LORA, d), 0.5 * C_DECAY_LORA ** -0.5)
    inputs['c_a0'] = nrm((N_C, d), 0.3)
    inputs['c_a1'] = nrm((N_C, d, C_ICLR_LORA), d ** -0.5)
    inputs['c_a2'] = nrm((N_C, C_ICLR_LORA, d), 0.5 * C_ICLR_LORA ** -0.5)
    inputs['c_g1'] = nrm((N_C, d, C_GATE_LORA), d ** -0.5)
    inputs['c_g2'] = nrm((N_C, C_GATE_LORA, d), C_GATE_LORA ** -0.5)
    inputs['c_k_k'] = 0.85 + nrm((N_C, d), 0.05)
    inputs['c_k_a'] = 1.0 + nrm((N_C, d), 0.05)
    inputs['c_r_k'] = nrm((N_C, C_HEADS, C_HEAD_DIM), 0.1)
    inputs['c_ln_w'] = 1.0 + nrm((N_C, d), 0.05)
    inputs['c_ln_b'] = nrm((N_C, d), 0.01)
    inputs['c_w_out'] = nrm((N_C, d, d), d ** -0.5)
    inputs['w_ffn_up'] = nrm((DEPTH, d, 2 * FFN_HIDDEN), d ** -0.5)
    inputs['w_ffn_down'] = nrm((DEPTH, FFN_HIDDEN, d), FFN_HIDDEN ** -0.5)
    return inputs


def reference(x_prompt, x_sample, cache_a1_kv, cache_a2_kv, cache_a3_kv, state_b, state_c_wkv, state_c_shift,
              norm_gains, w_in_ab, w_out_ab, b_lower_bounds, b_norm_gain, c_mu, c_w_rkv, c_w0, c_w1, c_w2,
              c_a0, c_a1, c_a2, c_g1, c_g2, c_k_k, c_k_a, c_r_k, c_ln_w, c_ln_b, c_w_out, w_ffn_up, w_ffn_down):
    p = dict(norm_gains=norm_gains, w_in_ab=w_in_ab, w_out_ab=w_out_ab, b_lower_bounds=b_lower_bounds,
             b_norm_gain=b_norm_gain, c_mu=c_mu, c_w_rkv=c_w_rkv, c_w0=c_w0, c_w1=c_w1, c_w2=c_w2,
             c_a0=c_a0, c_a1=c_a1, c_a2=c_a2, c_g1=c_g1, c_g2=c_g2, c_k_k=c_k_k, c_k_a=c_k_a, c_r_k=c_r_k,
             c_ln_w=c_ln_w, c_ln_b=c_ln_b, c_w_out=c_w_out, w_ffn_up=w_ffn_up, w_ffn_down=w_ffn_down)
    y_prompt, (pa1, pa2, pa3), pb, pwkv, pshift = _trunk(x_prompt, None, None, None, None, p)
    y_sample, (sa1, sa2, sa3), sb, swkv, sshift = _trunk(
        x_sample, (cache_a1_kv, cache_a2_kv, cache_a3_kv), state_b, state_c_wkv, state_c_shift, p)
    return (y_prompt, y_sample, pa1, pa2, pa3, pb, pwkv, pshift, sa1, sa2, sa3, sb, swkv, sshift)
```

```python
import numpy as np
from contextlib import ExitStack
import concourse.bass as bass
import concourse.mybir as mybir
from concourse.bass_utils import run_bass_kernel_spmd

F32 = mybir.dt.float32
BF16 = mybir.dt.bfloat16
AF = mybir.ActivationFunctionType
ALU = mybir.AluOpType
AX = mybir.AxisListType

D = 2048
KC = 16
NP = 2048
NS = 8
NT = NP + NS
HP = 2064
NEXT = NT + 4
FH = 5632
FKC = 44
EPS = 1e-6
GN_EPS = 64e-5
NEG = -30000.0
import os
NO_COPY = os.environ.get('NO_COPY') == '1'
DEBUG_LINES = os.environ.get('DEBUG_LINES') == '1'
DEBUG_MAP = {}
NOBUMP = ("cmask", "accN", "accD", "xt", "yt", "src", "htile", "T1", "T2", "T3", "T4", "R", "AV", "otok", "osq", "hf", "hb", "ho",
          "stg", "pbias", "sbias", "sc", "tmp", "rstd", "sq", "wstage", "aout", "yTr", "gtok", "ytok", "eg", "ebl", "bon", "st1", "st2",
          "st3", "rowb", "Sout", "Sin", "shf", "sho", "sD", "hr", "hsq", "kc_f", "vc_f", "rn", "hg")
ALLOC_LOG = {}
NHEADS = int(os.environ.get('NHEADS', '8'))
SKIP = os.environ.get('SKIP', '').split(',')


class Ev:
    __slots__ = ("sem", "val", "eng")

    def __init__(self, sem, val, eng):
        self.sem = sem
        self.val = val
        self.eng = eng


class T:
    def __init__(self, handle, name):
        self.h = handle
        self.name = name
        self.w = None
        self.r = []
        self.dsem = None
        self.dcount = 0

    def __getitem__(self, idx):
        return self.h[idx]


class Kern:
    ENGS = ("pe", "act", "dve", "pool", "sp")

    def __init__(self, arena_words, same_engine_sync=True):
        self.nc = bass.Bass("TRN2", target_bir_lowering=False)
        self.es = ExitStack()
        self.streams = {e: [] for e in self.ENGS}
        self.count = {e: 0 for e in self.ENGS}
        self.sems = {e: self.es.enter_context(self.nc.semaphore("s_" + e)) for e in self.ENGS}
        self.known = {e: {} for e in self.ENGS}
        self.pending = {e: [] for e in self.ENGS}
        self.same_engine_sync = same_engine_sync
        self.dsems = []
        self.dsem_rr = 0
        self.final_waits = {}
        self.arena = self.es.enter_context(self.nc.sbuf_tensor("arena", [128, arena_words], F32))
        self.arena_words = arena_words
        self.arena_base = int(self.nc.lookup_mloc(self.arena).addr)
        self.arena_off = 0
        self.n_dsem_max = 150
        self.live = []
        self.free_dsems = []

    def sb(self, name, shape, dt=F32):
        return T(self.es.enter_context(self.nc.sbuf_tensor(name, list(shape), dt)), name)

    def ps(self, name, shape, dt=F32):
        t = T(self.es.enter_context(self.nc.psum_tensor(name, list(shape), dt)), name)
        t.psum = True
        return t

    def alloc(self, name, shape, dt=F32):
        n = int(np.prod(shape[1:]))
        words = (n * (2 if dt == BF16 else 4) + 3) // 4
        words = (words + 7) // 8 * 8
        a0 = self.arena_base + self.arena_off * 4
        a1 = a0 + words * 4 - 1
        if a0 // 65536 != a1 // 65536:
            if name.startswith(("wt", "wb", "wg", "wu", "W6_", "l2c")):
                self.arena_off += ((-a0) % 256) // 4
            elif words * 4 <= 9000 and not name.startswith(NOBUMP):
                self.arena_off += ((-a0) % 65536) // 4
        assert self.arena_off + words <= self.arena_words, (name, self.arena_off, words, self.arena_words)
        v = self.arena[0:shape[0], self.arena_off:self.arena_off + words]
        if dt == BF16:
            v = v.bitcast(BF16)[:, 0:n]
        else:
            v = v[:, 0:n]
        if len(shape) == 3:
            v = v.rearrange("p (a b) -> p a b", b=shape[2])
        elif len(shape) == 4:
            v = v.rearrange("p (a b c) -> p a b c", b=shape[2], c=shape[3])
        t = T(v, name)
        ALLOC_LOG[name] = (self.arena_off, tuple(shape), "bf16" if dt == BF16 else "f32")
        self.live.append((t, self.arena_off))
        self.arena_off += words
        return t

    def reset_to(self, off):
        self.barrier()
        keep = []
        for t, o in self.live:
            if o >= off:
                if t.dsem is not None:
                    self.free_dsems.append(t.dsem)
                    t.dsem = None
            else:
                keep.append((t, o))
        self.live = keep
        self.arena_off = off

    def arena_reset(self):
        self.reset_to(0)

    def new_epoch(self):
        self.barrier()
        self.epoch = getattr(self, "epoch", 0) + 1
        for e in ("pe", "act", "dve", "pool"):
            if self.count[e] > 0:
                self.sems[e] = self.es.enter_context(self.nc.semaphore("s_%s_e%d" % (e, self.epoch)))
                self.count[e] = 0

    def dram_in(self, name, shape, dt=F32):
        return self.nc.dram_tensor(name, list(shape), dt, kind="ExternalInput").ap()

    def dram_out(self, name, shape, dt=F32):
        return self.nc.dram_tensor(name, list(shape), dt, kind="ExternalOutput").ap()

    def dram_scratch(self, name, shape, dt=F32):
        return T(self.nc.dram_tensor(name, list(shape), dt, kind="Internal").ap(), name)

    def _dsem(self, t):
        if t.dsem is None:
            if self.free_dsems:
                t.dsem = self.free_dsems.pop()
            elif len(self.dsems) < self.n_dsem_max:
                s = self.es.enter_context(self.nc.semaphore("d%d" % len(self.dsems)))
                t.dsem = [s, 0]
                self.dsems.append(t.dsem)
            else:
                raise RuntimeError("out of DMA semaphores")
        return t.dsem

    def _deps(self, eng, reads, writes, is_dma=False):
        deps = []
        for t in reads:
            if t.w is not None:
                deps.append(t.w)
            if getattr(t, "psum", False):
                deps.extend(ev for ev in t.r if ev.eng != eng)
        for t in writes:
            if t.w is not None:
                if not (is_dma and t.w.eng == "dma"):
                    deps.append(t.w)
            deps.extend(t.r)
        best = {}
        for ev in deps:
            if ev.eng == eng and (eng == "pe" or not self.same_engine_sync):
                continue
            assert ev.val is not None, "unresolved event used cross-engine"
            k = id(ev.sem)
            if k not in best or best[k].val < ev.val:
                best[k] = ev
        out = []
        kn = self.known[eng]
        for k, ev in best.items():
            if kn.get(k, -1) >= ev.val:
                continue
            kn[k] = ev.val
            out.append(ev)
        return out

    def op(self, eng, fn, reads=(), writes=(), inc=True):
        if DEBUG_LINES:
            import sys as _sys
            f = _sys._getframe(1)
            while f.f_code.co_name in ("mm", "tr", "act", "op"):
                f = f.f_back
            fn._line = f.f_lineno
        st = self.streams[eng]
        for ev in self._deps(eng, reads, writes):
            st.append(("wait", ev.sem, ev.val))
        if inc:
            self.count[eng] += 1
            ev = Ev(self.sems[eng], self.count[eng], eng)
            for pe in self.pending[eng]:
                pe.val = ev.val
            self.pending[eng] = []
        else:
            ev = Ev(self.sems[eng], None, eng)
            self.pending[eng].append(ev)
        st.append(("op", fn, inc, self.sems[eng]))
        for t in reads:
            t.r.append(ev)
            if len(t.r) > 64:
                t.r = t.r[-64:] if False else t.r
        for t in writes:
            t.w = ev
            t.r = []
        return ev

    def dma(self, q, out_ap, in_ap, reads=(), writes=(), sem_tile=None, **kw):
        st = self.streams[q]
        if sem_tile is None:
            sem_tile = writes[0] if writes else reads[0]
        for ev in self._deps(q, reads, writes, is_dma=True):
            st.append(("wait", ev.sem, ev.val))
        ds = self._dsem(sem_tile)
        ds[1] += 1
        ev = Ev(ds[0], 16 * ds[1], "dma")
        st.append(("dma", out_ap, in_ap, ds[0], kw))
        for t in reads:
            t.r.append(ev)
        for t in writes:
            t.w = ev
            t.r = []
        return ev

    def wait_final(self, ev):
        k = id(ev.sem)
        if k not in self.final_waits or self.final_waits[k].val < ev.val:
            self.final_waits[k] = ev

    def barrier(self):
        for e in self.ENGS:
            assert not self.pending[e], "pending non-inc ops at barrier"
        evs = [Ev(self.sems[e], self.count[e], e) for e in self.ENGS if self.count[e] > 0]
        evs += [Ev(ds[0], 16 * ds[1], "dma") for ds in self.dsems if ds[1] > 0]
        for e in self.ENGS:
            kn = self.known[e]
            for ev in evs:
                if ev.eng == e:
                    continue
                k = id(ev.sem)
                if kn.get(k, -1) >= ev.val:
                    continue
                kn[k] = ev.val
                self.streams[e].append(("wait", ev.sem, ev.val))

    def mm(self, out_t, out_ap, lhsT_ap, rhs_ap, reads, start=True, stop=True, inc=None):
        if lhsT_ap.tensor.name == "arena":
            esz = 2 if lhsT_ap.dtype == BF16 else 4
            pstride = self.arena_words * 4 // esz
            start_e = lhsT_ap.offset % pstride
            ext = 1 + sum((cnt - 1) * st for st, cnt in list(lhsT_ap.ap)[1:])
            b0 = self.arena_base + start_e * esz
            b1 = self.arena_base + (start_e + ext) * esz - 1
            assert b0 // 65536 == b1 // 65536, ("lhsT crosses 64KiB SBUF boundary", b0, b1)
        return self.op("pe", lambda e: e.matmul(out_ap, lhsT_ap, rhs_ap, start=start, stop=stop),
                       reads=reads, writes=[out_t], inc=(stop if inc is None else inc))

    def tr(self, out_t, out_ap, in_ap, ident_ap, reads):
        return self.op("pe", lambda e: e.transpose(out_ap, in_ap, ident_ap), reads=reads, writes=[out_t])

    def act(self, out_ap, in_ap, func, reads, writes, **kw):
        return self.op("act", lambda e: e.activation(out_ap, in_ap, func, **kw), reads=reads, writes=writes)

    def build(self):
        nc = self.nc
        for ev in self.final_waits.values():
            self.streams["sp"].append(("wait", ev.sem, ev.val))
        streams = self.streams
        sems = self.sems

        def replay(engname, eng):
            for item in streams[engname]:
                if item[0] == "wait":
                    eng.wait_ge(item[1], item[2])
                elif item[0] == "op":
                    ins = item[1](eng)
                    if DEBUG_LINES:
                        try:
                            DEBUG_MAP[str(ins.ins.name)] = getattr(item[1], "_line", None)
                        except Exception:
                            pass
                    if item[2]:
                        ins.then_inc(item[3], 1)
                else:
                    _, o, i, dsem, kw = item
                    eng.dma_start(out=o, in_=i, **kw).then_inc(dsem, 16)

        with nc.Block() as block:
            @block.tensor
            def _(e):
                replay("pe", e)

            @block.scalar
            def _(e):
                replay("act", e)

            @block.vector
            def _(e):
                replay("dve", e)

            @block.gpsimd
            def _(e):
                replay("pool", e)

            @block.sync
            def _(e):
                replay("sp", e)
        self.es.close()
        return nc


TILES = [[(0, 512)], [(512, 512)], [(1024, 512)], [(1536, 512), (2048, 8)]]


def ext0(u0):
    return 2 + u0 if u0 < NP else 4 + u0


def alibi_slopes():
    return np.exp2(-8.0 * np.arange(1, 9, dtype=np.float32) / 8).astype(np.float32)


A_GROUPS = ((128, 1), (512, 4), (2048, 16))


def host_consts():
    c = {}
    c["ident"] = np.eye(128, dtype=np.float32)
    c["ones"] = np.ones((128, 128), np.float32)
    bo = np.zeros((128, 128), np.float32)
    bo[:64, :64] = 1
    bo[64:, 64:] = 1
    c["blockones"] = bo
    sl = alibi_slopes()
    j = np.arange(128)[:, None]
    i = np.arange(128)[None, :]
    pb = np.zeros((8, 3, 2, 128, 128), np.float32)
    for h in range(8):
        for g, (win, dil) in enumerate(A_GROUPS):
            dist_cur = (i - j)
            pb[h, g, 1] = np.where(dist_cur >= 0, -sl[h] * dist_cur * dil, NEG)
            dist_prev = (i - j + 128)
            pb[h, g, 0] = np.where(dist_prev <= 128, -sl[h] * dist_prev * dil, NEG)
    c["pbias"] = np.ascontiguousarray(pb.transpose(0, 3, 1, 2, 4).reshape(8, 128, 6 * 128))
    sb = np.full((8, 24, 128, 8), NEG, np.float32)
    l = np.arange(8)[None, :]
    for h in range(8):
        bi = 0
        for g, (win, dil) in enumerate(A_GROUPS):
            n_buf = win
            for blk in range(n_buf // 128 + 1):
                idx = blk * 128 + np.arange(128)[:, None]
                dist = n_buf + l - idx
                ok = (dist >= 0) & (dist % dil == 0) & (dist // dil <= 128) & (idx < n_buf + 8)
                sb[h, bi] = np.where(ok, -sl[h] * dist, NEG)
                bi += 1
    c["sbias"] = np.ascontiguousarray(sb.transpose(0, 2, 1, 3).reshape(8, 128, 24 * 8))
    m = np.zeros((128, 128), np.float32)
    s = np.arange(64)[:, None]
    t = np.arange(64)[None, :]
    m[:64, :64] = (s <= t)
    c["hmask"] = m
    rm = np.zeros((128, 128), np.float32)
    for a in range(2):
        rm[a * 64:(a + 1) * 64, 0:64] = (s < t)
        rm[a * 64:(a + 1) * 64, 64:128] = (s <= t)
    c["rmask"] = rm
    cm = np.ones((128, NT), np.float32)
    cm[:, 0:NP:64] = 0.0
    cm[:, NP] = 0.0
    c["cmask"] = cm
    r2 = np.zeros((64, 2, 128), np.float32)
    r2[:, :, 0:64] = (s < t)[:, None, :]
    r2[:, :, 64:128] = (s <= t)[:, None, :]
    c["rmask2"] = r2.reshape(64, 256)
    md = np.zeros((64, 2, 64), np.float32)
    md[:, 0, :] = ((s // 32) == (t // 32))
    md[:, 1, :] = (s < 32) & (t >= 32)
    c["mDO"] = md.reshape(64, 128)
    return c


def blockify(W):
    Kd, N = W.shape
    return np.ascontiguousarray(W.reshape(Kd // 128, 128, N // 128, 128).transpose(2, 1, 0, 3))


def colvec(v):
    return np.ascontiguousarray(v.reshape(-1, 128).T)


def build_program(n_layers=4, debug=False):
    K = Kern(arena_words=50400)
    nc = K.nc
    xT_in = K.dram_in("xT_in", [128, KC, NT])
    gains_in = K.dram_in("gains", [128, 16, KC])
    consts = {}
    for nm, shp in (("ident", [128, 128]), ("ones", [128, 128]), ("blockones", [128, 128]), ("pbias", [8, 128, 768]),
                    ("sbias", [8, 128, 192]), ("hmask", [128, 128]), ("rmask", [128, 128]), ("cmask", [128, NT])):
        consts[nm] = K.dram_in("c_" + nm, shp)
    w_in = K.dram_in("w_in", [2, 104, 128, KC, 128])
    w_out = K.dram_in("w_out", [2, 16, 128, KC, 128])
    w_up = K.dram_in("w_up", [4, 88, 128, KC, 128])
    w_dn = K.dram_in("w_dn", [4, 16, 128, FKC, 128])
    lb_in = K.dram_in("lb", [128, 2, 8])
    bgain_in = K.dram_in("bgain", [128, 2, 8])
    cache_in = [K.dram_in("cache%d" % g, [2, A_GROUPS[g][0], 2, 8, 128]) for g in range(3)]
    stb_in = K.dram_in("state_b", [2, 8, 128, 128])
    yT_out = K.dram_out("yT_out", [128, KC, NT])
    pkv_out = [K.dram_out("pkv%d" % g, [2, min(A_GROUPS[g][0], NP), 2, 8, 128]) for g in range(3)]
    pb_out = K.dram_out("pb_out", [2, 8, 128, 128])
    skv_out = [K.dram_out("skv%d" % g, [2, A_GROUPS[g][0], 2, 8, 128]) for g in range(3)]
    sb_out = K.dram_out("sb_out", [2, 8, 128, 128])
    w_rkv = K.dram_in("w_rkv", [2, 3, 16, 128, KC, 128])
    w_cout = K.dram_in("w_cout", [2, 16, 128, KC, 128])
    w_l1 = K.dram_in("w_l1", [2, 4, 128, KC, 128])
    w_l2 = K.dram_in("w_l2", [2, 16, 128, 4, 128])
    mu_in = K.dram_in("mu", [128, 2, 6, KC])
    cvec_in = K.dram_in("cvec", [64, 2, 8, 32])
    rowb_in = K.dram_in("rowb", [2, 32, 64, 2, 64])
    cshift_in = K.dram_in("cshift", [128, 2, KC])
    cwkv_in = K.dram_in("cwkv", [2, 32, 64, 64])
    rmask2_in = K.dram_in("c_rmask2", [64, 256])
    pwkv_out = K.dram_out("pwkv_out", [2, 32, 64, 64])
    swkv_out = K.dram_out("swkv_out", [2, 32, 64, 64])
    pshift_out = K.dram_out("pshift_out", [128, 2, KC])
    sshift_out = K.dram_out("sshift_out", [128, 2, KC])
    dbg = {}
    if debug:
        dbg["ab"] = K.dram_out("dbg_ab", [16, 128, NT], BF16)

    xres = [K.dram_scratch("xres%d" % i, [128, KC, sum(w for _, w in TILES[i])]) for i in range(4)]
    ab_scr = [K.dram_scratch("ab%d" % i, [128, NT], BF16) for i in range(16)]
    act_scr = [K.dram_scratch("act%d" % i, [128, NT], BF16) for i in range(FKC)]
    h_scr = K.dram_scratch("h_scr", [128, KC, NEXT], BF16)

    ident_bf = K.sb("ident_bf", [128, 128], BF16)
    ident_f = K.sb("ident_f", [128, 128])
    ones_bf = K.sb("ones_bf", [128, 128], BF16)
    bones_bf = K.sb("bones_bf", [128, 128], BF16)
    hmask = K.sb("hmask_sb", [128, 128])
    rmask = K.sb("rmask_sb", [128, 128])
    gains = K.sb("gains_sb", [128, 16, KC])
    lb_raw = K.sb("lb_raw_sb", [128, 2, 8])
    lbt = K.sb("lbt", [128, 2, 8])
    oml = K.sb("oml", [128, 2, 8])
    bgain = K.sb("bgain_sb", [128, 2, 8])
    PS = [K.ps("ps%d" % i, [128, 512]) for i in range(8)]
    mu_sb = K.sb("mu_sb", [128, 2, 6, KC])
    omm_sb = K.sb("omm_sb", [128, 2, 6, KC])
    cvec = K.sb("cvec_sb", [64, 2, 8, 32])
    rmask2 = K.sb("rmask2_sb", [64, 2, 128])
    mDO = K.sb("mDO_sb", [64, 2, 64])
    mDO_in = K.dram_in("c_mDO", [64, 128])
    K.dma("sp", mDO[:], mDO_in.rearrange("p (a b) -> p a b", b=64), writes=[mDO])
    K.dma("sp", mu_sb[:], mu_in, writes=[mu_sb])
    K.dma("sp", cvec[:], cvec_in, writes=[cvec])
    K.dma("sp", rmask2[:], rmask2_in.rearrange("p (a b) -> p a b", b=128), writes=[rmask2])
    K.op("dve", lambda e: e.tensor_scalar(omm_sb[:], mu_sb[:], -1.0, 1.0, ALU.mult, ALU.add), reads=[mu_sb], writes=[omm_sb])
    K.op("dve", lambda e: e.tensor_scalar(cvec[:, :, 4, :], cvec[:, :, 3, :], -1.0, 1.0, ALU.mult, ALU.add), reads=[cvec], writes=[cvec])

    K.dma("sp", ident_f[:], consts["ident"], writes=[ident_f])
    ctmp = K.sb("ctmp", [128, 128])
    for dst_, nm_ in ((ident_bf, "ident"), (ones_bf, "ones"), (bones_bf, "blockones")):
        K.dma("sp", ctmp[:], consts[nm_], writes=[ctmp])
        K.op("dve", lambda e, dst_=dst_: e.tensor_copy(dst_[:], ctmp[:]), reads=[ctmp], writes=[dst_])
    K.dma("sp", hmask[:], consts["hmask"], writes=[hmask])
    K.dma("sp", rmask[:], consts["rmask"], writes=[rmask])
    K.dma("sp", gains[:], gains_in, writes=[gains])
    K.dma("sp", lb_raw[:], lb_in, writes=[lb_raw])
    K.dma("sp", bgain[:], bgain_in, writes=[bgain])
    K.op("dve", lambda e: e.memset(lbt[:, 0, :], 0.0), writes=[lbt])
    K.op("dve", lambda e: e.tensor_sub(lbt[:, 1, :], lb_raw[:, 1, :], lb_raw[:, 0, :]), reads=[lb_raw], writes=[lbt])
    K.act(lbt[:, 1, :], lbt[:, 1, :], AF.Sigmoid, reads=[lbt], writes=[lbt])
    K.op("dve", lambda e: e.tensor_scalar(oml[:], lbt[:], -1.0, 1.0, ALU.mult, ALU.add), reads=[lbt], writes=[oml])

    wq = [0]
    wstage = []

    def alloc_wstage(n, words=2816):
        wstage.clear()
        wstage.extend(K.alloc("wstage%d" % i, [128, words]) for i in range(n))

    def load_w(dst_t, dst_ap, src_ap):
        kcn = src_ap.shape[1]
        step = 22 if kcn > 22 else kcn
        for k0 in range(0, kcn, step):
            st = wstage[wq[0] % len(wstage)]
            wq[0] += 1
            sv = st[:, 0:step * 128].rearrange("p (a b) -> p a b", b=128)
            K.dma("sp", sv, src_ap[:, k0:k0 + step, :], writes=[st])
            K.op("dve", lambda e, sv=sv, k0=k0, step=step: e.tensor_copy(dst_ap[:, k0:k0 + step, :], sv), reads=[st], writes=[dst_t])

    def rstd_from_ss(ss_t, ss_ap, out_t, out_ap, n):
        K.act(out_ap, ss_ap, AF.Sqrt, reads=[ss_t], writes=[out_t], bias=EPS, scale=1.0 / n)
        K.op("dve", lambda e: e.reciprocal(out_ap, out_ap), reads=[out_t], writes=[out_t])

    def norm_to_h(xt, tile, hT, gidx, sqb, rstd, ss_ps):
        Wt = sum(w for _, w in tile)
        for c in range(KC):
            sq = sqb[c % 2]
            K.act(sq[:, 0:Wt], xt[:, c, 0:Wt], AF.Square, reads=[xt], writes=[sq])
            o = 0
            for (u0, w) in tile:
                pst = ss_ps[0] if w > 8 else ss_ps[1]
                K.mm(pst, pst[:, 0:w], ones_bf[:], sq[:, o:o + w], reads=[ones_bf, sq], start=(c == 0), stop=(c == KC - 1), inc=True)
                o += w
        o = 0
        for (u0, w) in tile:
            pst = ss_ps[0] if w > 8 else ss_ps[1]
            rstd_from_ss(pst, pst[:, 0:w], rstd, rstd[:, o:o + w], D)
            o += w
        Wt_ = sum(w for _, w in tile)
        for c in range(KC):
            K.op("dve", lambda e, c=c: e.scalar_tensor_tensor(
                hT[:, c, 0:Wt_], xt[:, c, 0:Wt_], gains[:, gidx, c:c + 1], rstd[:, 0:Wt_], ALU.mult, ALU.mult),
                reads=[xt, gains, rstd], writes=[hT])
        o = 0
        for (u0, w) in tile:
            e0 = ext0(u0)
            K.dma("sp", h_scr[:, :, e0:e0 + w], hT[:, :, o:o + w], reads=[hT], writes=[h_scr], sem_tile=hT)
            o += w

    def proj_tail(src_loader, KCn, wblocks, g_post, g_next, final=False):
        xt = K.alloc("xt", [128, KC, 520])
        yt = K.alloc("yt", [128, KC, 520], BF16)
        src = K.alloc("src", [128, KCn, 520], BF16)
        wb = [K.alloc("wb%d" % i, [128, KCn, 128], BF16) for i in range(2)]
        sqb = [K.alloc("sq%d" % i, [128, 520], BF16) for i in range(2)]
        rstd = K.alloc("rstd", [128, 520])
        tmp = [K.alloc("tmp%d" % i, [128, 520]) for i in range(2)]
        hT = K.alloc("htile", [128, KC, 520], BF16)
        alloc_wstage(2)
        for ti, tile in enumerate(TILES):
            Wt = sum(w for _, w in tile)
            K.dma("sp", xt[:, :, 0:Wt], xres[ti][:, :, :], reads=[xres[ti]], writes=[xt])
            src_loader(src, tile)
            for c in range(KC):
                wbt = wb[c % 2]
                load_w(wbt, wbt[:], wblocks[c])
                o = 0
                sq = sqb[c % 2]
                for si, (u0, w) in enumerate(tile):
                    pst = PS[(c % 2) * 2 + si]
                    for kc in range(KCn):
                        K.mm(pst, pst[:, 0:w], wbt[:, kc, :], src[:, kc, o:o + w], reads=[wbt, src], start=(kc == 0), stop=(kc == KCn - 1))
                    K.act(yt[:, c, o:o + w], pst[:, 0:w], AF.Copy, reads=[pst], writes=[yt])
                    K.act(sq[:, o:o + w], pst[:, 0:w], AF.Square, reads=[pst], writes=[sq])
                    sst = PS[4 + si]
                    K.mm(sst, sst[:, 0:w], ones_bf[:], sq[:, o:o + w], reads=[ones_bf, sq], start=(c == 0), stop=(c == KC - 1), inc=True)
                    o += w
            o = 0
            for si, (u0, w) in enumerate(tile):
                rstd_from_ss(PS[4 + si], PS[4 + si][:, 0:w], rstd, rstd[:, o:o + w], D)
                o += w
            for c in range(KC):
                tm = tmp[c % 2]
                K.op("dve", lambda e, c=c, tm=tm: e.scalar_tensor_tensor(tm[:, 0:Wt], yt[:, c, 0:Wt], gains[:, g_post, c:c + 1], rstd[:, 0:Wt], ALU.mult, ALU.mult),
                     reads=[yt, gains, rstd], writes=[tm])
                K.op("dve", lambda e, c=c, tm=tm: e.tensor_tensor(xt[:, c, 0:Wt], xt[:, c, 0:Wt], tm[:, 0:Wt], ALU.add), reads=[xt, tm], writes=[xt])
            if final:
                o = 0
                for (u0, w) in tile:
                    K.wait_final(K.dma("sp", yT_out[:, :, u0:u0 + w], xt[:, :, o:o + w], reads=[xt], sem_tile=xt))
                    o += w
            else:
                K.dma("sp", xres[ti][:, :, :], xt[:, :, 0:Wt], reads=[xt], writes=[xres[ti]], sem_tile=xt)
                norm_to_h(xt, tile, hT, g_next, sqb, rstd, [PS[6], PS[7]])

    K.arena_reset()
    mark0 = 0

    arena_base = int(K.nc.lookup_mloc(K.arena).addr)
    hpad_words = ((65536 - arena_base) % (HP * 2)) // 4
    assert (65536 - arena_base) % (HP * 2) % 4 == 0

    def load_hT(pre=()):
        assert K.arena_off == 0
        pre_tiles = [K.alloc(nm, shp, dt) for (nm, shp, dt) in pre]
        assert K.arena_off <= hpad_words, (K.arena_off, hpad_words)
        K.arena_off = hpad_words
        hT = K.alloc("hT", [128, KC, HP], BF16)
        load_hT.pre = pre_tiles
        K.op("dve", lambda e: e.memset(hT[:, :, 0:2], 0.0), writes=[hT])
        K.op("dve", lambda e: e.memset(hT[:, :, NP + 2:NP + 4], 0.0), writes=[hT])
        for c0 in range(0, KC, 4):
            K.dma("sp", hT[:, c0:c0 + 4, 2:NP + 2], h_scr[:, c0:c0 + 4, 2:NP + 2], reads=[h_scr], writes=[hT])
        K.dma("sp", hT[:, :, NP + 4:NEXT], h_scr[:, :, NP + 4:NEXT], reads=[h_scr], writes=[hT])
        return hT

    def stage0():
        xt = K.alloc("xt0", [128, KC, 520])
        hT = K.alloc("htile0", [128, KC, 520], BF16)
        sqb = [K.alloc("sq0%d" % i, [128, 520], BF16) for i in range(2)]
        rstd = K.alloc("rstd0", [128, 520])
        for ti, tile in enumerate(TILES):
            o = 0
            for (u0, w) in tile:
                K.dma("sp", xt[:, :, o:o + w], xT_in[:, :, u0:u0 + w], writes=[xt])
                o += w
            Wt = o
            K.dma("sp", xres[ti][:, :, :], xt[:, :, 0:Wt], reads=[xt], writes=[xres[ti]], sem_tile=xt)
            norm_to_h(xt, tile, hT, 0, sqb, rstd, [PS[6], PS[7]])
    stage0()

    def stage_reset():
        K.reset_to(mark0)

    def renorm(gidx):
        stage_reset()
        xt = K.alloc("xtr", [128, KC, 520])
        hTt = K.alloc("htiler", [128, KC, 520], BF16)
        sqb = [K.alloc("sqr%d" % i, [128, 520], BF16) for i in range(2)]
        rstd = K.alloc("rstdr", [128, 520])
        for ti, tile in enumerate(TILES):
            Wt = sum(w for _, w in tile)
            K.dma("sp", xt[:, :, 0:Wt], xres[ti][:, :, :], reads=[xres[ti]], writes=[xt])
            norm_to_h(xt, tile, hTt, gidx, sqb, rstd, [PS[6], PS[7]])

    def even_mixer(li):
        stage_reset()
        hT = load_hT()
        wt = K.alloc("wt", [128, 9, KC, 128], BF16)
        aout = K.alloc("aout", [128, NT], BF16)
        cmask = K.alloc("cmask", [128, NT])
        K.dma("sp", cmask[:], consts["cmask"], writes=[cmask])
        alloc_wstage(1, 2048)
        cpy = T(None, "cpy")
        mark1 = K.arena_off

        def alloc_attn():
            K.reset_to(mark1)
            d = {}
            d["qT"] = [K.alloc("qT%d" % g, [128, NT], BF16) for g in range(3)]
            d["kT"] = [K.alloc("kT%d" % g, [128, NT], BF16) for g in range(3)]
            d["vtok"] = [K.alloc("vtok%d" % g, [128, 16, 128], BF16) for g in range(3)]
            d["vs"] = K.alloc("vs", [8, 3, 128], BF16)
            d["stg"] = [K.alloc("stg%d" % i, [128, 4, 128]) for i in range(2)]
            d["accN"] = K.alloc("accN", [128, NP])
            d["accD"] = K.alloc("accD", [128, NP])
            d["pbias"] = K.alloc("pbias", [128, 3, 2, 128])
            d["sbias"] = K.alloc("sbias", [128, 24, 8])
            d["sc"] = [K.alloc("sc%d" % i, [128, 128]) for i in range(2)]
            d["pT"] = [K.alloc("pT%d" % i, [128, 128], BF16) for i in range(2)]
            d["kc_b"] = [K.alloc("kc_b%d" % i, [128, 128], BF16) for i in range(2)]
            d["vc_b"] = [K.alloc("vc_b%d" % i, [128, 128], BF16) for i in range(2)]
            d["kcT"] = [K.alloc("kcT%d" % i, [128, 128], BF16) for i in range(2)]
            d["kc_f"] = [K.alloc("kc_f%d" % i, [128, 128]) for i in range(2)]
            d["vc_f"] = [K.alloc("vc_f%d" % i, [128, 128]) for i in range(2)]
            d["sD"] = K.alloc("sD", [128, 8])
            return d

        def alloc_hgrn():
            K.reset_to(mark1)
            d = {}
            d["hq"] = K.alloc("hq", [128, NT], BF16)
            d["hg"] = K.alloc("hg", [128, NT], BF16)
            d["hf"] = K.alloc("hf", [128, NT])
            d["hb"] = K.alloc("hb", [128, NT])
            d["ho"] = K.alloc("ho", [128, NT])
            d["qt_"] = K.alloc("qtl", [128, NT], BF16)
            d["kt_"] = K.alloc("ktl", [128, NT], BF16)
            d["ebl"] = K.alloc("ebl", [128, 40])
            d["vtk"] = K.alloc("vtk", [64, 33, 128], BF16)
            d["S"] = K.alloc("S", [128, 128])
            d["Sb"] = K.alloc("Sb", [128, 128], BF16)
            d["S2"] = K.alloc("S2", [128, 128])
            d["S2b"] = K.alloc("S2b", [128, 128], BF16)
            d["ktok"] = [K.alloc("ktok%d" % i, [64, 128], BF16) for i in range(2)]
            d["attm"] = [K.alloc("attm%d" % i, [64, 64], BF16) for i in range(2)]
            d["hsq"] = K.alloc("hsq", [128, 520], BF16)
            d["hr"] = K.alloc("hr", [128, 520])
            return d

        def proj_fm(dst_t, dst_fn, blk, evac):
            n = 0
            for tile in TILES:
                for (u0, w) in tile:
                    pst = PS[n % 2]
                    n += 1
                    e0 = ext0(u0)
                    for kc in range(KC):
                        K.mm(pst, pst[:, 0:w], wt[:, blk, kc, :], hT[:, kc, e0:e0 + w], reads=[wt, hT], start=(kc == 0), stop=(kc == KC - 1))
                    evac(pst, w, u0)

        def proj_tm(blk, cols_list, pst, width=128):
            for i, (e0, step, ntok) in enumerate(cols_list):
                for kc in range(KC):
                    K.mm(pst, pst[0:ntok, i * 128:(i + 1) * 128], hT[:, kc, e0:e0 + (ntok - 1) * step + 1:step], wt[:, blk, kc, :],
                         reads=[wt, hT], start=(kc == 0), stop=(kc == KC - 1))

        for h in range(NHEADS):
            A_ = alloc_attn()
            qT, kT, vtok, vs, stg, accN, accD, pbias, sbias, sc, pT, kc_b, vc_b, kcT, sD, kc_f, vc_f = (A_[k] for k in
                ("qT", "kT", "vtok", "vs", "stg", "accN", "accD", "pbias", "sbias", "sc", "pT", "kc_b", "vc_b", "kcT", "sD", "kc_f", "vc_f"))
            for j in range(9):
                load_w(wt, wt[:, j, :, :], w_in[li, j * 8 + h])
            K.dma("sp", pbias[:], consts["pbias"][h].rearrange("p (g a i) -> p g a i", g=3, a=2), writes=[pbias])
            K.dma("sp", sbias[:], consts["sbias"][h].rearrange("p (b l) -> p b l", l=8), writes=[sbias])
            for g in (range(3) if 'pfm' not in SKIP else ()):
                proj_fm(qT[g], None, 3 * g, lambda pst, w, u0, g=g: K.act(qT[g][:, u0:u0 + w], pst[:, 0:w], AF.Copy, reads=[pst], writes=[qT[g]]))
                proj_fm(kT[g], None, 3 * g + 1, lambda pst, w, u0, g=g: K.op("dve", lambda e: e.tensor_copy(kT[g][:, u0:u0 + w], pst[:, 0:w]), reads=[pst], writes=[kT[g]]))
            for g, (win, dil) in enumerate(A_GROUPS if 'ptm' not in SKIP else ()):
                if 'g1' in SKIP and g >= 1:
                    continue
                nblk = NP // dil // 128
                first_out = NP - min(win, NP)
                for grp in range(4):
                    tiles4 = []
                    for i in range(4):
                        idx = grp * 4 + i
                        r, n = idx // nblk, idx % nblk
                        tiles4.append((r, n, 2 + n * 128 * dil + r))
                    for which in (2, 1):
                        pst = PS[2 + (which - 1)]
                        need_out = [(n * 128 * dil + r) >= first_out for (r, n, _) in tiles4]
                        if which == 1 and (not any(need_out) or 'kproj' in SKIP):
                            continue
                        proj_tm(3 * g + which, [(e0, dil, 128) for (_, _, e0) in tiles4], pst)
                        st = stg[which - 1]
                        if which == 2:
                            K.op("dve", lambda e, pst=pst, g=g, grp=grp: e.tensor_copy(vtok[g][:, grp * 4:grp * 4 + 4, :], pst[:, :].rearrange("p (a b) -> p a b", b=128)),
                                 reads=[pst], writes=[vtok[g]])
                        if any(need_out):
                            K.act(st[:], pst[:, :].rearrange("p (a b) -> p a b", b=128), AF.Copy, reads=[pst], writes=[st])
                            for i, (r, n, _) in enumerate(tiles4):
                                if not need_out[i]:
                                    continue
                                t0 = n * 128 * dil + r - first_out
                                if 'kvout' not in SKIP:
                                    K.wait_final(K.dma("sp", pkv_out[g][li, t0:t0 + 127 * dil + 1:dil, which - 1, h, :], st[:, i, :], reads=[st], sem_tile=st))
                for which in ((2, 1) if 'smp' not in SKIP else ()):
                    pst = PS[2 + (which - 1)]
                    proj_tm(3 * g + which, [(ext0(NP), 1, 8)], pst)
                    st = stg[which - 1]
                    K.act(st[0:8, 0, :], pst[0:8, 0:128], AF.Copy, reads=[pst], writes=[st])
                    if which == 2:
                        K.op("dve", lambda e, pst=pst, g=g: e.tensor_copy(vs[0:8, g, :], pst[0:8, 0:128]), reads=[pst], writes=[vs])
                    if 'kvout' not in SKIP:
                        K.wait_final(K.dma("sp", skv_out[g][li, win - 8:win, which - 1, h, :], st[0:8, 0, :], reads=[st], sem_tile=st))
                    if h == 0 and which == 1 and not NO_COPY:
                        K.wait_final(K.dma("sp", skv_out[g][li, 0:win - 8], cache_in[g][li, 8:win], sem_tile=cpy))
            un = 0
            for g, (win, dil) in enumerate(A_GROUPS if 'att' not in SKIP else ()):
                nblk = NP // dil // 128
                for r in range(dil):
                    for n in range(nblk):
                        q0 = n * 128 * dil + r
                        kbs = [n - 1, n] if n > 0 else [n]
                        pN, pD = PS[4 + 2 * (un % 2)], PS[5 + 2 * (un % 2)]
                        un += 1
                        for ki, kb in enumerate(kbs):
                            k0 = kb * 128 * dil + r
                            pS = PS[2 + (ki % 2)]
                            K.mm(pS, pS[:, 0:128], kT[g][:, k0:k0 + 127 * dil + 1:dil], qT[g][:, q0:q0 + 127 * dil + 1:dil], reads=[kT[g], qT[g]])
                            which = 1 if kb == n else 0
                            s_ = sc[ki % 2]
                            p_ = pT[ki % 2]
                            K.op("dve", lambda e, pS=pS, s_=s_, g=g, which=which: e.scalar_tensor_tensor(s_[:], pS[:, 0:128], 128 ** -0.5, pbias[:, g, which, :], ALU.mult, ALU.add),
                                 reads=[pS, pbias], writes=[s_])
                            K.act(p_[:], s_[:], AF.Exp, reads=[s_], writes=[p_])
                            K.mm(pN, pN[:, 0:128], vtok[g][:, r * nblk + kb, :], p_[:], reads=[vtok[g], p_], start=(ki == 0), stop=(ki == len(kbs) - 1))
                            K.mm(pD, pD[:, 0:128], ones_bf[:], p_[:], reads=[ones_bf, p_], start=(ki == 0), stop=(ki == len(kbs) - 1))
                        sl_ = slice(q0, q0 + 127 * dil + 1, dil)
                        if g == 0:
                            K.act(accN[:, sl_], pN[:, 0:128], AF.Copy, reads=[pN], writes=[accN])
                            K.op("dve", lambda e, pD=pD, sl_=sl_: e.tensor_copy(accD[:, sl_], pD[:, 0:128]), reads=[pD], writes=[accD])
                        else:
                            K.op("dve", lambda e, pN=pN, sl_=sl_: e.tensor_tensor(accN[:, sl_], accN[:, sl_], pN[:, 0:128], ALU.add), reads=[pN, accN], writes=[accN])
                            K.op("dve", lambda e, pD=pD, sl_=sl_: e.tensor_tensor(accD[:, sl_], accD[:, sl_], pD[:, 0:128], ALU.add), reads=[pD, accD], writes=[accD])
            K.op("dve", lambda e: e.reciprocal(accD[:], accD[:]), reads=[accD], writes=[accD])
            K.op("dve", lambda e: e.tensor_tensor(aout[:, 0:NP], accN[:], accD[:], ALU.mult), reads=[accN, accD], writes=[aout])
            pN, pD = PS[4], PS[5]
            bi = 0
            nblocks_total = 24
            for g, (win, dil) in enumerate(A_GROUPS if 'satt' not in SKIP else ()):
                for blk in range(win // 128 + 1):
                    new = (blk == win // 128)
                    nk_ = 8 if new else 128
                    kb_, vb_, kT_ = kc_b[bi % 2], vc_b[bi % 2], kcT[bi % 2]
                    pS = PS[2 + (bi % 2)]
                    if not new:
                        kf_, vf_ = kc_f[bi % 2], vc_f[bi % 2]
                        K.dma("sp", kf_[:], cache_in[g][li, blk * 128:(blk + 1) * 128, 0, h, :], writes=[kf_])
                        K.dma("sp", vf_[:], cache_in[g][li, blk * 128:(blk + 1) * 128, 1, h, :], writes=[vf_])
                        K.op("dve", lambda e, kb_=kb_, kf_=kf_: e.tensor_copy(kb_[:], kf_[:]), reads=[kf_], writes=[kb_])
                        K.op("dve", lambda e, vb_=vb_, vf_=vf_: e.tensor_copy(vb_[:], vf_[:]), reads=[vf_], writes=[vb_])
                        pTr = PS[0 + (bi % 2)]
                        trv = pTr[:, :].bitcast(BF16)[:, 0:128]
                        K.tr(pTr, trv, kb_[:], ident_bf[:], reads=[kb_, ident_bf])
                        K.op("dve", lambda e, kT_=kT_, trv=trv: e.tensor_copy(kT_[:], trv), reads=[pTr], writes=[kT_])
                        K.mm(pS, pS[0:128, 0:8], kT_[:], qT[g][:, NP:NT], reads=[kT_, qT[g]])
                        vl = vb_[:]
                    else:
                        K.mm(pS, pS[0:8, 0:8], kT[g][:, NP:NT], qT[g][:, NP:NT], reads=[kT[g], qT[g]])
                        vl = vs[0:8, g, :]
                    s_ = sc[bi % 2]
                    p_ = pT[bi % 2]
                    K.op("dve", lambda e, pS=pS, s_=s_, bi=bi, nk_=nk_: e.scalar_tensor_tensor(s_[0:nk_, 0:8], pS[0:nk_, 0:8], 128 ** -0.5, sbias[0:nk_, bi, :], ALU.mult, ALU.add),
                         reads=[pS, sbias], writes=[s_])
                    K.act(p_[0:nk_, 0:8], s_[0:nk_, 0:8], AF.Exp, reads=[s_], writes=[p_])
                    rd = [p_, vs] if new else [p_, vb_]
                    K.mm(pN, pN[:, 0:8], vl, p_[0:nk_, 0:8], reads=rd, start=(bi == 0), stop=(bi == nblocks_total - 1), inc=True)
                    K.mm(pD, pD[:, 0:8], ones_bf[0:nk_, :], p_[0:nk_, 0:8], reads=[ones_bf, p_], start=(bi == 0), stop=(bi == nblocks_total - 1), inc=True)
                    bi += 1
            K.op("dve", lambda e: e.reciprocal(sD[:], pD[:, 0:8]), reads=[pD], writes=[sD])
            K.op("dve", lambda e: e.tensor_tensor(aout[:, NP:NT], pN[:, 0:8], sD[:], ALU.mult), reads=[pN, sD], writes=[aout])
            K.dma("sp", ab_scr[h][:, :], aout[:], reads=[aout], writes=[ab_scr[h]], sem_tile=aout)

            if 'hgrn' in SKIP:
                continue
            H_ = alloc_hgrn()
            hq, hg, hf, hb, ho, qt_, kt_, ebl, vtk, S, Sb, S2, S2b, ktok, attm, hsq, hr = (H_[k] for k in
                ("hq", "hg", "hf", "hb", "ho", "qt_", "kt_", "ebl", "vtk", "S", "Sb", "S2", "S2b", "ktok", "attm", "hsq", "hr"))
            for j in range(4):
                load_w(wt, wt[:, j, :, :], w_in[li, (9 + j) * 8 + h])
            proj_fm(hq, None, 0, lambda pst, w, u0: K.act(hq[:, u0:u0 + w], pst[:, 0:w], AF.Silu, reads=[pst], writes=[hq]))
            proj_fm(hf, None, 1, lambda pst, w, u0: K.act(hf[:, u0:u0 + w], pst[:, 0:w], AF.Sigmoid, reads=[pst], writes=[hf]))
            proj_fm(hg, None, 3, lambda pst, w, u0: K.act(hg[:, u0:u0 + w], pst[:, 0:w], AF.Silu, reads=[pst], writes=[hg]))
            for grp in range(8):
                pst = PS[2 + grp % 2]
                for i in range(4):
                    ch = grp * 4 + i
                    for kc in range(KC):
                        K.mm(pst, pst[0:64, i * 128:(i + 1) * 128], hT[:, kc, 2 + ch * 64:2 + ch * 64 + 64], wt[:, 2, kc, :], reads=[wt, hT], start=(kc == 0), stop=(kc == KC - 1))
                K.op("dve", lambda e, pst=pst, grp=grp: e.tensor_copy(vtk[:, grp * 4:grp * 4 + 4, :], pst[0:64, :].rearrange("p (a b) -> p a b", b=128)), reads=[pst], writes=[vtk])
            pst = PS[2]
            for kc in range(KC):
                K.mm(pst, pst[0:8, 0:128], hT[:, kc, ext0(NP):ext0(NP) + 8], wt[:, 2, kc, :], reads=[wt, hT], start=(kc == 0), stop=(kc == KC - 1))
            K.op("dve", lambda e, pst=pst: e.tensor_copy(vtk[0:8, 32, :], pst[0:8, 0:128]), reads=[pst], writes=[vtk])
            K.op("dve", lambda e, h=h: e.tensor_scalar(hf[:], hf[:], oml[:, li, h:h + 1], lbt[:, li, h:h + 1], ALU.mult, ALU.add), reads=[hf, oml, lbt], writes=[hf])
            K.act(hb[:], hf[:], AF.Ln, reads=[hf], writes=[hb])
            K.op("dve", lambda e: e.tensor_scalar(hf[:], hf[:], -1.0, 1.0, ALU.mult, ALU.add), reads=[hf], writes=[hf])
            K.op("dve", lambda e: e.tensor_tensor_scan(ho[:], cmask[:], hb[:], 0.0, ALU.mult, ALU.add), reads=[cmask, hb], writes=[ho])
            K.act(hb[:], ho[:], AF.Exp, reads=[ho], writes=[hb])
            K.op("dve", lambda e: e.tensor_tensor(qt_[:], hq[:], hb[:], ALU.mult), reads=[hq, hb], writes=[qt_])
            K.op("dve", lambda e: e.tensor_copy(ebl[:, 0:32], hb[:, 63:NP:64]), reads=[hb], writes=[ebl])
            K.op("dve", lambda e: e.tensor_copy(ebl[:, 32:33], hb[:, NT - 1:NT]), reads=[hb], writes=[ebl])
            K.act(hb[:], ho[:], AF.Exp, reads=[ho], writes=[hb], scale=-1.0)
            K.op("dve", lambda e: e.tensor_tensor(kt_[:], hf[:], hb[:], ALU.mult), reads=[hf, hb], writes=[kt_])
            K.op("dve", lambda e: e.memset(S[:], 0.0), writes=[S])
            K.op("dve", lambda e: e.memset(Sb[:], 0.0), writes=[Sb])
            K.dma("sp", S2[:], stb_in[li, h], writes=[S2])
            K.op("dve", lambda e: e.tensor_copy(S2b[:], S2[:]), reads=[S2], writes=[S2b])
            for ch in range(33):
                smp = (ch == 32)
                C = 8 if smp else 64
                c0 = NP if smp else ch * 64
                St, Sbt = (S2, S2b) if smp else (S, Sb)
                kk_ = ktok[ch % 2]
                am_ = attm[ch % 2]
                pTr = PS[0 + (ch % 2)]
                trv = pTr[:, :].bitcast(BF16)
                K.tr(pTr, trv[0:C, 0:128], kt_[:, c0:c0 + C], ident_bf[:], reads=[kt_, ident_bf])
                K.act(kk_[0:C, :], trv[0:C, 0:128], AF.Copy, reads=[pTr], writes=[kk_])
                pA = PS[2 + (ch % 2)]
                K.mm(pA, pA[0:C, 0:C], kt_[:, c0:c0 + C], qt_[:, c0:c0 + C], reads=[kt_, qt_])
                K.op("dve", lambda e, pA=pA, am_=am_, C=C: e.tensor_tensor(am_[0:C, 0:C], pA[0:C, 0:C], hmask[0:C, 0:C], ALU.mult), reads=[pA, hmask], writes=[am_])
                pO = PS[4 + (ch % 2)]
                K.mm(pO, pO[:, 0:C], Sbt[:], qt_[:, c0:c0 + C], reads=[Sbt, qt_], start=True, stop=False)
                K.mm(pO, pO[:, 0:C], vtk[0:C, ch, :], am_[0:C, 0:C], reads=[vtk, am_], start=False, stop=True)
                K.act(ho[:, c0:c0 + C], pO[:, 0:C], AF.Copy, reads=[pO], writes=[ho])
                pP = PS[6 + (ch % 2)]
                K.mm(pP, pP[:, 0:128], kk_[0:C, :], vtk[0:C, ch, :], reads=[kk_, vtk])
                K.op("dve", lambda e, St=St, ch=ch: e.tensor_scalar(St[:], St[:], ebl[:, ch:ch + 1], None, ALU.mult), reads=[St, ebl], writes=[St])
                K.op("dve", lambda e, St=St, pP=pP, ch=ch: e.scalar_tensor_tensor(St[:], pP[:, 0:128], ebl[:, ch:ch + 1], St[:], ALU.mult, ALU.add), reads=[St, pP, ebl], writes=[St])
                K.act(Sbt[:], St[:], AF.Copy, reads=[St], writes=[Sbt])
            K.wait_final(K.dma("sp", pb_out[li, h], S[:], reads=[S], sem_tile=S))
            K.wait_final(K.dma("sp", sb_out[li, h], S2[:], reads=[S2], sem_tile=S2))
            for tile in TILES:
                Wt = sum(w for _, w in tile)
                u0 = tile[0][0]
                K.act(hsq[:, 0:Wt], ho[:, u0:u0 + Wt], AF.Square, reads=[ho], writes=[hsq])
                o = 0
                for si, (uu, w) in enumerate(tile):
                    pst = PS[si]
                    K.mm(pst, pst[:, 0:w], ones_bf[:], hsq[:, o:o + w], reads=[ones_bf, hsq])
                    rstd_from_ss(pst, pst[:, 0:w], hr, hr[:, o:o + w], 128)
                    o += w
                K.op("dve", lambda e, u0=u0, Wt=Wt, h=h: e.scalar_tensor_tensor(hr[:, 0:Wt], ho[:, u0:u0 + Wt], bgain[:, li, h:h + 1], hr[:, 0:Wt], ALU.mult, ALU.mult),
                     reads=[ho, bgain, hr], writes=[hr])
                K.op("dve", lambda e, u0=u0, Wt=Wt: e.tensor_tensor(aout[:, u0:u0 + Wt], hr[:, 0:Wt], hg[:, u0:u0 + Wt], ALU.mult), reads=[hr, hg], writes=[aout])
            K.dma("sp", ab_scr[8 + h][:, :], aout[:], reads=[aout], writes=[ab_scr[8 + h]], sem_tile=aout)


    def rwkv_mixer(li):
        stage_reset()
        hT = load_hT(pre=(("rn", [64, 520], F32), ("sqs", [64, 520], BF16)))
        rn, sqs = load_hT.pre
        shf = K.alloc("shf", [128, KC])
        sho = K.alloc("sho", [128, 2, KC])
        K.dma("sp", shf[:], cshift_in[:, li, :], writes=[shf])
        K.op("dve", lambda e: e.tensor_copy(hT[:, :, NP + 3], shf[:]), reads=[shf], writes=[hT])
        K.op("dve", lambda e: e.tensor_copy(sho[:, 0, :], hT[:, :, NP + 1]), reads=[hT], writes=[sho])
        K.op("dve", lambda e: e.tensor_copy(sho[:, 1, :], hT[:, :, NEXT - 1]), reads=[hT], writes=[sho])
        K.wait_final(K.dma("sp", pshift_out[:, li, :], sho[:, 0, :], reads=[sho], sem_tile=sho))
        K.wait_final(K.dma("sp", sshift_out[:, li, :], sho[:, 1, :], reads=[sho], sem_tile=sho))
        lo = K.alloc("lo", [128, 4, NT], BF16)
        W6 = [K.alloc("W6_%d" % i, [128, KC, 128], BF16) for i in range(6)]
        l2c = K.alloc("l2c", [128, 4, 128], BF16)
        cmask = K.alloc("cmaskr", [64, NT], BF16)
        alloc_wstage(1, 2048)
        for a_, b_ in ((0, 2048), (2048, NT)):
            K.dma("sp", wstage[0][0:64, 0:b_ - a_], consts["cmask"][0:64, a_:b_], writes=[wstage[0]])
            K.op("dve", lambda e, a_=a_, b_=b_, ws=wstage[0]: e.tensor_copy(cmask[:, a_:b_], ws[0:64, 0:b_ - a_]), reads=[wstage[0]], writes=[cmask])

        def load_w_mu(dA, dB, j_mu, src_ap):
            st = wstage[0]
            sv = st[:, 0:KC * 128].rearrange("p (a b) -> p a b", b=128)
            K.dma("sp", sv, src_ap, writes=[st])
            for kc in range(KC):
                K.op("dve", lambda e, kc=kc: e.tensor_scalar(dA[:, kc, :], sv[:, kc, :], omm_sb[:, li, j_mu, kc:kc + 1], None, ALU.mult),
                     reads=[st, omm_sb], writes=[dA])
                K.act(dB[:, kc, :], sv[:, kc, :], AF.Copy, reads=[st, mu_sb], writes=[dB], scale=mu_sb[:, li, j_mu, kc:kc + 1])

        def proj_shift(dA, dB, col0, M, evac):
            n = 0
            for tile in TILES:
                for (u0, w) in tile:
                    pst = PS[n % 2]
                    n += 1
                    e0 = ext0(u0)
                    for kc in range(KC):
                        K.mm(pst, pst[0:M, 0:w], dA[:, kc, col0:col0 + M], hT[:, kc, e0:e0 + w], reads=[dA, hT], start=(kc == 0), stop=False)
                        K.mm(pst, pst[0:M, 0:w], dB[:, kc, col0:col0 + M], hT[:, kc, e0 - 1:e0 - 1 + w], reads=[dB, hT], start=False, stop=(kc == KC - 1))
                    evac(pst, w, u0)

        for blk, j_mu, fn in ((0, 1, AF.Tanh), (1, 4, AF.Copy), (2, 5, AF.Sigmoid), (3, 5, AF.Sigmoid)):
            load_w_mu(W6[0], W6[1], j_mu, w_l1[li, blk])
            proj_shift(W6[0], W6[1], 0, 128, lambda pst, w, u0, blk=blk, fn=fn: K.act(lo[:, blk, u0:u0 + w], pst[:, 0:w], fn, reads=[pst], writes=[lo]))
        mark2 = K.arena_off

        for c in range(16):
            K.reset_to(mark2)
            l2v = wstage[0][:, 0:512].rearrange("p (a b) -> p a b", b=128)
            K.dma("sp", l2v, w_l2[li, c], writes=[wstage[0]])
            K.op("dve", lambda e, l2v=l2v: e.tensor_copy(l2c[:], l2v), reads=[wstage[0]], writes=[l2c])
            for j, j_mu in ((0, 0), (1, 2), (2, 3)):
                load_w_mu(W6[2 * j], W6[2 * j + 1], j_mu, w_rkv[li, j, c])
            for pi in range(2):
                hd = 2 * c + pi
                col0 = pi * 64
                K.reset_to(mark2)
                BK = K.alloc("BK", [64, 33, 2, 64], BF16)
                AR = K.alloc("AR", [64, 33, 2, 64], BF16)
                rk = K.alloc("rk", [64, 33, 64], BF16)
                vtok = K.alloc("vtokr", [64, 33, 64], BF16)
                eg = K.alloc("eg", [64, 40])
                mark3 = K.arena_off
                T1 = K.alloc("T1", [64, NT])
                T2 = K.alloc("T2", [64, NT])
                T3 = K.alloc("T3", [64, NT])
                R = K.alloc("R", [64, NT])
                T4 = K.alloc("T4", [64, NT])
                AV = K.alloc("AV", [64, NT], BF16)
                for t_ in (BK, AR, rk, vtok):
                    K.op("dve", lambda e, t_=t_: e.memset(t_[:, 32], 0.0), writes=[t_])

                cvs = [cvec[:, li, w_, hd:hd + 1] for w_ in range(8)]

                def cv(which, cvs=cvs):
                    return cvs[which]

                def chunked(t3d, slot=None):
                    if slot is None:
                        return t3d[:, 0:32, :], t3d[:, 32, 0:8]
                    return t3d[:, 0:32, slot, :], t3d[:, 32, slot, 0:8]

                def flat(t2d):
                    return t2d[:, 0:NP].rearrange("p (a b) -> p a b", b=64), t2d[:, NP:NT]

                def ew2(out3, a2, b2, op, eng="dve"):
                    (oa, ob, _), (aa, ab_), (ba, bb) = out3, flat(a2[0]), flat(b2[0])
                    K.op(eng, lambda e: e.tensor_tensor(oa, aa, ba, op), reads=[a2[1], b2[1]], writes=[out3[2]])
                    K.op(eng, lambda e: e.tensor_tensor(ob, ab_, bb, op), reads=[a2[1], b2[1]], writes=[out3[2]])

                n = 0
                for tile in TILES:
                    for (u0, w) in tile:
                        pst = PS[2 + n % 2]
                        n += 1
                        K.mm(pst, pst[0:64, 0:w], l2c[:, 0, col0:col0 + 64], lo[:, 0, u0:u0 + w], reads=[l2c, lo])
                        K.act(T1[:, u0:u0 + w], pst[0:64, 0:w], AF.Sigmoid, reads=[pst, cvec], writes=[T1], bias=cv(0))
                K.op("dve", lambda e: e.tensor_scalar(T1[:], T1[:], -0.6065306597, None, ALU.mult), reads=[T1], writes=[T1])
                K.op("dve", lambda e: e.tensor_tensor_scan(T2[:], cmask[:], T1[:], 0.0, ALU.mult, ALU.add), reads=[cmask, T1], writes=[T2])
                K.op("dve", lambda e: e.tensor_tensor(T1[:], T2[:], T1[:], ALU.subtract), reads=[T1, T2], writes=[T1])
                K.act(T1[:], T1[:], AF.Exp, reads=[T1], writes=[T1])
                K.act(T3[:], T2[:], AF.Exp, reads=[T2], writes=[T3])
                proj_shift(W6[0], W6[1], col0, 64, lambda pst, w, u0: K.act(R[:, u0:u0 + w], pst[0:64, 0:w], AF.Copy, reads=[pst], writes=[R]))
                oa, ob = chunked(AR, 1)
                ew2((oa, ob, AR), (R, R), (T3, T3), ALU.mult)
                K.op("dve", lambda e: e.tensor_copy(eg[:, 0:32], T3[:, 63:NP:64]), reads=[T3], writes=[eg])
                K.op("dve", lambda e: e.tensor_copy(eg[:, 32:33], T3[:, NT - 1:NT]), reads=[T3], writes=[eg])
                K.act(T3[:], T2[:], AF.Exp, reads=[T2], writes=[T3], scale=-1.0)
                proj_shift(W6[2], W6[3], col0, 64, lambda pst, w, u0: K.act(T2[:, u0:u0 + w], pst[0:64, 0:w], AF.Copy, reads=[pst], writes=[T2]))
                n = 0
                for tile in TILES:
                    for (u0, w) in tile:
                        pst = PS[2 + n % 2]
                        n += 1
                        K.mm(pst, pst[0:64, 0:w], l2c[:, 1, col0:col0 + 64], lo[:, 1, u0:u0 + w], reads=[l2c, lo])
                        K.act(AV[:, u0:u0 + w], pst[0:64, 0:w], AF.Sigmoid, reads=[pst, cvec], writes=[AV], bias=cv(1))
                K.op("dve", lambda e, c2=cv(2): e.tensor_scalar(T4[:], T2[:], c2, None, ALU.mult), reads=[T2, cvec], writes=[T4])
                for tile in TILES:
                    Wt = sum(w for _, w in tile)
                    u0 = tile[0][0]
                    K.act(sqs[:, 0:Wt], T4[:, u0:u0 + Wt], AF.Square, reads=[T4], writes=[sqs])
                    o = 0
                    for si, (uu, w) in enumerate(tile):
                        pst = PS[4 + si]
                        K.mm(pst, pst[0:64, 0:w], ones_bf[0:64, 0:64], sqs[:, o:o + w], reads=[ones_bf, sqs])
                        K.act(rn[:, o:o + w], pst[0:64, 0:w], AF.Sqrt, reads=[pst], writes=[rn], bias=1e-24, scale=1.0)
                        o += w
                    K.op("dve", lambda e, Wt=Wt: e.reciprocal(rn[:, 0:Wt], rn[:, 0:Wt]), reads=[rn], writes=[rn])
                    K.op("dve", lambda e, Wt=Wt, u0=u0: e.tensor_tensor(T4[:, u0:u0 + Wt], T4[:, u0:u0 + Wt], rn[:, 0:Wt], ALU.mult), reads=[T4, rn], writes=[T4])
                oa, ob = chunked(AR, 0)
                (aa, ab_), (ba, bb) = flat(T4), flat(T1)
                K.op("dve", lambda e, oa=oa, aa=aa, ba=ba: e.scalar_tensor_tensor(oa, aa, -1.0, ba, ALU.mult, ALU.mult), reads=[T4, T1], writes=[AR])
                K.op("dve", lambda e, ob=ob, ab_=ab_, bb=bb: e.scalar_tensor_tensor(ob, ab_, -1.0, bb, ALU.mult, ALU.mult), reads=[T4, T1], writes=[AR])
                K.op("dve", lambda e, c3=cv(3), c4=cv(4): e.tensor_scalar(T1[:], AV[:], c3, c4, ALU.mult, ALU.add), reads=[AV, cvec], writes=[T1])
                K.op("dve", lambda e: e.tensor_tensor(T2[:], T2[:], T1[:], ALU.mult), reads=[T2, T1], writes=[T2])
                oa, ob = chunked(rk)
                (aa, ab_), (ba, bb) = flat(R), flat(T2)
                K.op("dve", lambda e, oa=oa, aa=aa, ba=ba, c5=cv(5): e.scalar_tensor_tensor(oa, aa, c5, ba, ALU.mult, ALU.mult), reads=[R, T2, cvec], writes=[rk])
                K.op("dve", lambda e, ob=ob, ab_=ab_, bb=bb, c5=cv(5): e.scalar_tensor_tensor(ob, ab_, c5, bb, ALU.mult, ALU.mult), reads=[R, T2, cvec], writes=[rk])
                oa, ob = chunked(BK, 1)
                ew2((oa, ob, BK), (T2, T2), (T3, T3), ALU.mult)
                K.op("dve", lambda e: e.tensor_tensor(T1[:], T4[:], AV[:], ALU.mult), reads=[T4, AV], writes=[T1])
                oa, ob = chunked(BK, 0)
                ew2((oa, ob, BK), (T1, T1), (T3, T3), ALU.mult)
                for grp in range(4):
                    pst = PS[6 + grp % 2]
                    for i in range(8):
                        ch = grp * 8 + i
                        e0 = 2 + ch * 64
                        for kc in range(KC):
                            K.mm(pst, pst[0:64, i * 64:(i + 1) * 64], hT[:, kc, e0:e0 + 64], W6[4][:, kc, col0:col0 + 64], reads=[W6[4], hT], start=(kc == 0), stop=False)
                            K.mm(pst, pst[0:64, i * 64:(i + 1) * 64], hT[:, kc, e0 - 1:e0 + 63], W6[5][:, kc, col0:col0 + 64], reads=[W6[5], hT], start=False, stop=(kc == KC - 1))
                    K.op("dve", lambda e, pst=pst, grp=grp: e.tensor_copy(vtok[:, grp * 8:grp * 8 + 8, :], pst[0:64, :].rearrange("p (a b) -> p a b", b=64)), reads=[pst], writes=[vtok])
                pst = PS[6]
                e0 = ext0(NP)
                for kc in range(KC):
                    K.mm(pst, pst[0:8, 0:64], hT[:, kc, e0:e0 + 8], W6[4][:, kc, col0:col0 + 64], reads=[W6[4], hT], start=(kc == 0), stop=False)
                    K.mm(pst, pst[0:8, 0:64], hT[:, kc, e0 - 1:e0 + 7], W6[5][:, kc, col0:col0 + 64], reads=[W6[5], hT], start=False, stop=(kc == KC - 1))
                K.op("dve", lambda e, pst=pst: e.tensor_copy(vtok[0:8, 32, :], pst[0:8, 0:64]), reads=[pst], writes=[vtok])

                K.reset_to(mark3)
                A = K.alloc("A", [64, 64])
                Ab = K.alloc("Ab", [64, 64], BF16)
                As = K.alloc("As", [64, 64])
                Asb = K.alloc("Asb", [64, 64], BF16)
                Sin = K.alloc("Sin", [64, 64])
                Gm = K.alloc("Gm", [64, 2, 128])
                Gmb = K.alloc("Gmb", [64, 2, 128], BF16)
                Pp = [K.alloc("Pp%d" % i, [64, 2, 64]) for i in range(2)]
                Tm = K.alloc("Tm", [64, 64])
                XT = K.alloc("XT", [64, 64])
                Ys = K.alloc("Ys", [64, 64])
                Zs = K.alloc("Zs", [64, 64])
                Mo = K.alloc("Mo", [64, 64])
                UT = K.alloc("UT", [64, 64], BF16)
                BKt = K.alloc("BKt", [64, 2, 64], BF16)
                otok = K.alloc("otok", [64, 33, 64])
                osq = K.alloc("osq", [64, 33, 64])
                gtok = K.alloc("gtok", [64, 33, 64], BF16)
                ytok = K.alloc("ytok", [64, 33, 64], BF16)
                yT = K.alloc("yTr", [64, NT], BF16)
                bon = K.alloc("bon", [64, 40])
                st1 = K.alloc("st1", [64, 40])
                st2 = K.alloc("st2", [64, 40])
                st3 = K.alloc("st3", [64, 40])
                rowb = K.alloc("rowb", [64, 2, 64])
                Sout = K.alloc("Sout", [64, 2, 64])
                K.dma("sp", rowb[:], rowb_in[li, hd], writes=[rowb])
                K.dma("sp", Sin[:], cwkv_in[li, hd], writes=[Sin])
                K.op("dve", lambda e: e.memset(A[:], 0.0), writes=[A])
                K.op("dve", lambda e: e.memset(Ab[:], 0.0), writes=[Ab])
                pI = PS[0]
                K.tr(pI, pI[0:64, 0:64], Sin[:], ident_f[0:64, 0:64], reads=[Sin, ident_f])
                K.op("dve", lambda e: e.tensor_copy(As[:], pI[0:64, 0:64]), reads=[pI], writes=[As])
                K.act(Asb[:], As[:], AF.Copy, reads=[As], writes=[Asb])
                idf = ident_f[0:64, 0:64]
                for ch in range(33):
                    At, Abt = (As, Asb) if ch == 32 else (A, Ab)
                    arf = AR[:, ch, :, :].rearrange("p a b -> p (a b)")
                    pG = PS[ch % 2]
                    K.mm(pG, pG[0:64, 0:128], BK[:, ch, 0, :], arf, reads=[BK, AR])
                    K.mm(pG, pG[0:64, 128:256], BK[:, ch, 1, :], arf, reads=[BK, AR])
                    K.op("dve", lambda e, pG=pG: e.tensor_tensor(Gm[:], pG[0:64, 0:256].rearrange("p (a b) -> p a b", b=128), rmask2[:], ALU.mult), reads=[pG, rmask2], writes=[Gm])
                    K.act(Gmb[:], Gm[:], AF.Copy, reads=[Gm], writes=[Gmb])
                    pT_ = PS[2]
                    K.op("dve", lambda e: e.tensor_tensor(Pp[0][:, 0, :], Gm[:, 0, 0:64], mDO[:, 0, :], ALU.mult), reads=[Gm, mDO], writes=[Pp[0]])
                    K.op("dve", lambda e: e.tensor_tensor(Mo[:], Gm[:, 0, 0:64], mDO[:, 1, :], ALU.mult), reads=[Gm, mDO], writes=[Mo])
                    K.tr(pT_, pT_[0:64, 0:64], Pp[0][:, 0, :], idf, reads=[Pp[0], ident_f])
                    K.act(Pp[0][:, 1, :], pT_[0:64, 0:64], AF.Copy, reads=[pT_], writes=[Pp[0]])
                    K.op("dve", lambda e: e.tensor_tensor(Tm[:], Pp[0][:, 0, :], idf, ALU.add), reads=[Pp[0], ident_f], writes=[Tm])
                    for it in range(4):
                        Pc, Pn = Pp[it % 2], Pp[(it + 1) % 2]
                        pX = PS[3]
                        K.mm(pX, pX[0:64, 0:64], Pc[:, 1, :], Pc[:, 0, :], reads=[Pc])
                        K.mm(pX, pX[0:64, 64:128], Pc[:, 0, :], Pc[:, 1, :], reads=[Pc])
                        K.act(Pn[:], pX[0:64, 0:128].rearrange("p (a b) -> p a b", b=64), AF.Copy, reads=[pX], writes=[Pn])
                        pZ = PS[4]
                        K.mm(pZ, pZ[0:64, 0:64], Pn[:, 1, :], Tm[:], reads=[Pn, Tm])
                        K.op("dve", lambda e, pZ=pZ: e.tensor_tensor(Tm[:], Tm[:], pZ[0:64, 0:64], ALU.add), reads=[Tm, pZ], writes=[Tm])
                    pW = PS[5]
                    K.mm(pW, pW[0:64, 0:64], AR[:, ch, 0, :], Abt[:], reads=[AR, Abt], start=True, stop=False)
                    K.mm(pW, pW[0:64, 0:64], Gmb[:, 1, 0:64], vtok[:, ch, :], reads=[Gmb, vtok], start=False, stop=True)
                    K.op("dve", lambda e, pW=pW: e.tensor_copy(XT[:], pW[0:64, 0:64]), reads=[pW], writes=[XT])
                    pU = PS[6]
                    K.mm(pU, pU[0:64, 0:64], Tm[:], XT[:], reads=[Tm, XT])
                    K.act(Ys[:], pU[0:64, 0:64], AF.Copy, reads=[pU], writes=[Ys])
                    K.mm(pU, pU[0:64, 64:128], Mo[:], Ys[:], reads=[Mo, Ys])
                    K.op("dve", lambda e, pU=pU: e.tensor_copy(Zs[:], pU[0:64, 64:128]), reads=[pU], writes=[Zs])
                    K.mm(pU, pU[0:64, 128:192], Tm[:], Zs[:], reads=[Tm, Zs])
                    K.op("dve", lambda e, pU=pU: e.tensor_tensor(UT[:], Ys[:], pU[0:64, 128:192], ALU.add), reads=[Ys, pU], writes=[UT])
                    pO = PS[7]
                    K.mm(pO, pO[0:64, 0:64], AR[:, ch, 1, :], Abt[:], reads=[AR, Abt], start=True, stop=False)
                    K.mm(pO, pO[0:64, 0:64], Gmb[:, 0, 64:128], UT[:], reads=[Gmb, UT], start=False, stop=False)
                    K.mm(pO, pO[0:64, 0:64], Gmb[:, 1, 64:128], vtok[:, ch, :], reads=[Gmb, vtok], start=False, stop=True)
                    K.act(otok[:, ch, :], pO[0:64, 0:64], AF.Copy, reads=[pO], writes=[otok])
                    pB = PS[5]
                    pBv = pB[:, :].bitcast(BF16)
                    K.tr(pB, pBv[0:64, 256:320], BK[:, ch, 0, :], ident_bf[0:64, 0:64], reads=[BK, ident_bf])
                    K.tr(pB, pBv[0:64, 320:384], BK[:, ch, 1, :], ident_bf[0:64, 0:64], reads=[BK, ident_bf])
                    K.op("dve", lambda e, pBv=pBv: e.tensor_copy(BKt[:], pBv[0:64, 256:384].rearrange("p (a b) -> p a b", b=64)), reads=[pB], writes=[BKt])
                    pD = PS[6]
                    K.mm(pD, pD[0:64, 256:320], BKt[:, 0, :], UT[:], reads=[BKt, UT], start=True, stop=False)
                    K.mm(pD, pD[0:64, 256:320], BKt[:, 1, :], vtok[:, ch, :], reads=[BKt, vtok], start=False, stop=True)
                    K.op("dve", lambda e, At=At, ch=ch: e.tensor_scalar(At[:], At[:], eg[:, ch:ch + 1], None, ALU.mult), reads=[At, eg], writes=[At])
                    K.op("dve", lambda e, At=At, ch=ch, pD=pD: e.scalar_tensor_tensor(At[:], pD[0:64, 256:320], eg[:, ch:ch + 1], At[:], ALU.mult, ALU.add), reads=[At, pD, eg], writes=[At])
                    K.act(Abt[:], At[:], AF.Copy, reads=[At], writes=[Abt])
                pF = PS[0]
                K.tr(pF, pF[0:64, 0:64], A[:], idf, reads=[A, ident_f])
                K.tr(pF, pF[0:64, 64:128], As[:], idf, reads=[As, ident_f])
                K.op("dve", lambda e: e.tensor_copy(Sout[:], pF[0:64, 0:128].rearrange("p (a b) -> p a b", b=64)), reads=[pF], writes=[Sout])
                K.wait_final(K.dma("sp", pwkv_out[li, hd], Sout[:, 0, :], reads=[Sout], sem_tile=Sout))
                K.wait_final(K.dma("sp", swkv_out[li, hd], Sout[:, 1, :], reads=[Sout], sem_tile=Sout))
                pBn = PS[1]
                for ch in range(33):
                    K.mm(pBn, pBn[0:64, ch:ch + 1], rk[:, ch, :], ones_bf[0:64, 0:1], reads=[rk, ones_bf])
                K.op("dve", lambda e: e.tensor_copy(bon[:, 0:33], pBn[0:64, 0:33]), reads=[pBn], writes=[bon])
                for grp in range(5):
                    pst = PS[2 + grp % 2]
                    nch = 8 if grp < 4 else 1
                    for i in range(nch):
                        ch = grp * 8 + i
                        cs, cw = (NP, 8) if ch == 32 else (ch * 64, 64)
                        K.mm(pst, pst[0:cw, i * 64:(i + 1) * 64], lo[:, 2, cs:cs + cw], l2c[:, 2, col0:col0 + 64], reads=[lo, l2c], start=True, stop=False)
                        K.mm(pst, pst[0:cw, i * 64:(i + 1) * 64], lo[:, 3, cs:cs + cw], l2c[:, 3, col0:col0 + 64], reads=[lo, l2c], start=False, stop=True)
                    if grp < 4:
                        K.act(gtok[:, grp * 8:grp * 8 + 8, :], pst[0:64, :].rearrange("p (a b) -> p a b", b=64), AF.Copy, reads=[pst], writes=[gtok])
                    else:
                        K.op("dve", lambda e: e.memset(gtok[:, 32, :], 0.0), writes=[gtok])
                        K.act(gtok[0:8, 32, :], pst[0:8, 0:64], AF.Copy, reads=[pst], writes=[gtok])
                K.op("dve", lambda e: e.tensor_reduce(st1[:, 0:33], otok[:], AX.X, ALU.add), reads=[otok], writes=[st1])
                K.op("dve", lambda e: e.tensor_tensor(osq[:], otok[:], otok[:], ALU.mult), reads=[otok], writes=[osq])
                K.op("dve", lambda e: e.tensor_reduce(st2[:, 0:33], osq[:], AX.X, ALU.add), reads=[osq], writes=[st2])
                K.op("dve", lambda e: e.tensor_scalar(st1[:, 0:33], st1[:, 0:33], 1.0 / 64, None, ALU.mult), reads=[st1], writes=[st1])
                K.op("dve", lambda e: e.tensor_tensor(st3[:, 0:33], st1[:, 0:33], st1[:, 0:33], ALU.mult), reads=[st1], writes=[st3])
                K.op("dve", lambda e: e.scalar_tensor_tensor(st2[:, 0:33], st2[:, 0:33], 1.0 / 64, st3[:, 0:33], ALU.mult, ALU.subtract), reads=[st2, st3], writes=[st2])
                K.act(st2[:, 0:33], st2[:, 0:33], AF.Sqrt, reads=[st2], writes=[st2], bias=GN_EPS, scale=1.0)
                K.op("dve", lambda e: e.reciprocal(st2[:, 0:33], st2[:, 0:33]), reads=[st2], writes=[st2])
                b3 = lambda t_: t_[:, 0:33].unsqueeze(2).to_broadcast([64, 33, 64])
                r3 = lambda j: rowb[:, j, :].unsqueeze(1).to_broadcast([64, 33, 64])
                K.op("dve", lambda e: e.tensor_tensor(osq[:], otok[:], b3(st1), ALU.subtract), reads=[otok, st1], writes=[osq])
                K.op("dve", lambda e: e.tensor_tensor(osq[:], osq[:], b3(st2), ALU.mult), reads=[osq, st2], writes=[osq])
                K.op("dve", lambda e: e.tensor_tensor(osq[:], osq[:], r3(0), ALU.mult), reads=[osq, rowb], writes=[osq])
                K.op("dve", lambda e: e.tensor_tensor(osq[:], osq[:], r3(1), ALU.add), reads=[osq, rowb], writes=[osq])
                K.op("dve", lambda e: e.tensor_tensor(otok[:], vtok[:], b3(bon), ALU.mult), reads=[vtok, bon], writes=[otok])
                K.op("dve", lambda e: e.tensor_tensor(osq[:], osq[:], otok[:], ALU.add), reads=[osq, otok], writes=[osq])
                K.op("dve", lambda e: e.tensor_tensor(ytok[:], osq[:], gtok[:], ALU.mult), reads=[osq, gtok], writes=[ytok])
                for grp in range(5):
                    pst = PS[4 + grp % 2]
                    pv = pst[:, :].bitcast(BF16)
                    nch = 8 if grp < 4 else 1
                    for i in range(nch):
                        K.tr(pst, pv[0:64, i * 64:(i + 1) * 64], ytok[:, grp * 8 + i, :], ident_bf[0:64, 0:64], reads=[ytok, ident_bf])
                    if grp < 4:
                        K.act(yT[:, grp * 512:(grp + 1) * 512], pv[0:64, 0:512], AF.Copy, reads=[pst], writes=[yT])
                    else:
                        K.act(yT[:, NP:NT], pv[0:64, 0:8], AF.Copy, reads=[pst], writes=[yT])
                K.dma("sp", ab_scr[c][col0:col0 + 64, :], yT[:], reads=[yT], writes=[ab_scr[c]], sem_tile=yT)

    def ab_loader(src, tile):
        o = 0
        for (u0, w) in tile:
            for c in range(16):
                K.dma("sp", src[:, c, o:o + w], ab_scr[c][:, u0:u0 + w], reads=[ab_scr[c]], writes=[src])
            o += w

    def act_loader(src, tile):
        o = 0
        for (u0, w) in tile:
            for c in range(FKC):
                K.dma("sp", src[:, c, o:o + w], act_scr[c][:, u0:u0 + w], reads=[act_scr[c]], writes=[src])
            o += w

    def ffn_up(layer):
        stage_reset()
        hT = load_hT()
        alloc_wstage(2, 2048)
        wg = [K.alloc("wg%d" % i, [128, KC, 128], BF16) for i in range(2)]
        wu = [K.alloc("wu%d" % i, [128, KC, 128], BF16) for i in range(2)]
        sg = [K.alloc("sg%d" % i, [128, 512]) for i in range(2)]
        arow = [K.alloc("arow%d" % i, [128, NT], BF16) for i in range(2)]
        for j in range(FKC):
            wg_, wu_ = wg[j % 2], wu[j % 2]
            load_w(wg_, wg_[:], w_up[layer, j])
            load_w(wu_, wu_[:], w_up[layer, FKC + j])
            ar = arow[j % 2]
            n = 0
            for tile in TILES:
                for (u0, w) in tile:
                    pg, pu = PS[(n % 2) * 2], PS[(n % 2) * 2 + 1]
                    s_ = sg[n % 2]
                    n += 1
                    e0 = ext0(u0)
                    for kc in range(KC):
                        K.mm(pg, pg[:, 0:w], wg_[:, kc, :], hT[:, kc, e0:e0 + w], reads=[wg_, hT], start=(kc == 0), stop=(kc == KC - 1))
                    for kc in range(KC):
                        K.mm(pu, pu[:, 0:w], wu_[:, kc, :], hT[:, kc, e0:e0 + w], reads=[wu_, hT], start=(kc == 0), stop=(kc == KC - 1))
                    K.act(s_[:, 0:w], pg[:, 0:w], AF.Silu, reads=[pg], writes=[s_])
                    K.op("dve", lambda e, ar=ar, s_=s_, pu=pu, u0=u0, w=w: e.tensor_tensor(ar[:, u0:u0 + w], s_[:, 0:w], pu[:, 0:w], ALU.mult), reads=[s_, pu], writes=[ar])
            K.dma("sp", act_scr[j][:, :], ar[:], reads=[ar], writes=[act_scr[j]], sem_tile=ar)

    for layer in range(n_layers):
        li = layer // 2
        if layer > 0:
            K.new_epoch()
        if layer % 2 == 0:
            if 'mixer' not in SKIP:
                even_mixer(li)
            if debug and layer == 0:
                for c in range(16):
                    K.wait_final(K.dma("sp", dbg["ab"][c], ab_scr[c][:, :], reads=[ab_scr[c]], sem_tile=ab_scr[c]))
            stage_reset()
            if 'tail' not in SKIP:
                proj_tail(ab_loader, KC, [w_out[li, c] for c in range(16)], layer * 4 + 1, layer * 4 + 2)
        else:
            rwkv_mixer(li)
            stage_reset()
            proj_tail(ab_loader, KC, [w_cout[li, c] for c in range(16)], layer * 4 + 1, layer * 4 + 2)
        if 'ffn' in SKIP:
            continue
        ffn_up(layer)
        stage_reset()
        last = (layer == n_layers - 1)
        proj_tail(act_loader, FKC, [w_dn[layer, c] for c in range(16)], layer * 4 + 3, (layer * 4 + 4) % 16, final=last)
    K.barrier()
    return K.build()


def prep_shared(inp):
    sh = dict(("c_" + k, v) for k, v in host_consts().items())
    ng = inp["norm_gains"]
    sh["gains"] = np.ascontiguousarray(ng.reshape(16, KC, 128).transpose(2, 0, 1))
    sh["w_in"] = np.stack([blockify(inp["w_in_ab"][l]) for l in range(2)])
    sh["w_out"] = np.stack([blockify(inp["w_out_ab"][l]) for l in range(2)])
    sh["w_up"] = np.stack([blockify(inp["w_ffn_up"][l]) for l in range(4)])
    sh["w_dn"] = np.stack([blockify(inp["w_ffn_down"][l]) for l in range(4)])
    sh["lb"] = np.ascontiguousarray(inp["b_lower_bounds"].reshape(2, 8, 128).transpose(2, 0, 1))
    sh["bgain"] = np.ascontiguousarray(inp["b_norm_gain"].reshape(2, 8, 128).transpose(2, 0, 1))
    sh["w_rkv"] = np.stack([np.stack([blockify(inp["c_w_rkv"][l, j]) for j in range(3)]) for l in range(2)])
    sh["w_cout"] = np.stack([blockify(inp["c_w_out"][l]) for l in range(2)])

    def padc(w):
        o = np.zeros((w.shape[0], 128), np.float32)
        o[:, :w.shape[1]] = w
        return o

    def padr(w):
        o = np.zeros((128, w.shape[1]), np.float32)
        o[:w.shape[0]] = w
        return o
    sh["w_l1"] = np.stack([blockify(np.concatenate([padc(inp["c_w1"][l]), padc(inp["c_a1"][l]), inp["c_g1"][l]], axis=1)) for l in range(2)])
    l2 = []
    for l in range(2):
        full = np.stack([padr(inp["c_w2"][l]), padr(inp["c_a2"][l]), inp["c_g2"][l][0:128], inp["c_g2"][l][128:256]], axis=1)
        l2.append(full.reshape(128, 4, 16, 128).transpose(2, 0, 1, 3))
    sh["w_l2"] = np.ascontiguousarray(np.stack(l2))
    sh["mu"] = np.ascontiguousarray(inp["c_mu"].reshape(2, 6, KC, 128).transpose(3, 0, 1, 2))
    cv = np.zeros((64, 2, 8, 32), np.float32)
    for i, nm in enumerate(("c_w0", "c_a0", "c_k_k", "c_k_a", None, "c_r_k")):
        if nm is not None:
            cv[:, :, i, :] = inp[nm].reshape(2, 32, 64).transpose(2, 0, 1)
    sh["cvec"] = cv
    rb = np.stack([inp["c_ln_w"].reshape(2, 32, 64), inp["c_ln_b"].reshape(2, 32, 64)], axis=2)
    sh["rowb"] = np.ascontiguousarray(np.broadcast_to(rb[:, :, None, :, :], (2, 32, 64, 2, 64)))
    return sh


def prep_core(inp, sh, core):
    pb_, sbi = core % 4, core
    m = dict(sh)
    x = np.concatenate([inp["x_prompt"][pb_], inp["x_sample"][sbi]], axis=0)
    m["xT_in"] = np.ascontiguousarray(x.T.reshape(KC, 128, NT).transpose(1, 0, 2))
    for g in range(3):
        m["cache%d" % g] = np.ascontiguousarray(inp["cache_a%d_kv" % (g + 1)][:, sbi])
    m["state_b"] = np.ascontiguousarray(inp["state_b"][:, sbi])
    m["cshift"] = np.ascontiguousarray(inp["state_c_shift"][:, sbi].reshape(2, KC, 128).transpose(2, 0, 1))
    m["cwkv"] = np.ascontiguousarray(inp["state_c_wkv"][:, sbi])
    return m


_NC_CACHE = {}


def kernel(**inp):
    inp = {k: np.asarray(v) for k, v in inp.items()}
    if "nc" not in _NC_CACHE:
        _NC_CACHE["nc"] = build_program(4)
    nc = _NC_CACHE["nc"]
    sh = prep_shared(inp)
    in_maps = [prep_core(inp, sh, c) for c in range(8)]
    res = run_bass_kernel_spmd(nc, in_maps, core_ids=list(range(8))).results

    def tok(r, a, b):
        return r["yT_out"][:, :, a:b].transpose(2, 1, 0).reshape(b - a, D)
    y_prompt = np.stack([tok(res[c], 0, NP) for c in range(4)]).astype(np.float32)
    y_sample = np.stack([tok(res[c], NP, NT) for c in range(8)]).astype(np.float32)
    pa = [np.stack([res[c]["pkv%d" % g] for c in range(4)], axis=1) for g in range(3)]
    pb = np.stack([res[c]["pb_out"] for c in range(4)], axis=1)
    sa = [np.stack([res[c]["skv%d" % g] for c in range(8)], axis=1) for g in range(3)]
    sb_ = np.stack([res[c]["sb_out"] for c in range(8)], axis=1)
    pwkv = np.stack([res[c]["pwkv_out"] for c in range(4)], axis=1)
    swkv = np.stack([res[c]["swkv_out"] for c in range(8)], axis=1)
    pshift = np.stack([res[c]["pshift_out"].transpose(1, 2, 0).reshape(2, D) for c in range(4)], axis=1)
    sshift = np.stack([res[c]["sshift_out"].transpose(1, 2, 0).reshape(2, D) for c in range(8)], axis=1)
    return (y_prompt, y_sample, pa[0], pa[1], pa[2], pb, pwkv, pshift, sa[0], sa[1], sa[2], sb_, swkv, sshift)
```
